# Optimizing a Trainium2 kernel written in Bass

```python
import math
import jax
import jax.numpy as jnp
from jax import lax
import numpy as np


D_MODEL = 1024
BATCH = 4
SEQ = 8192
DEPTH = 2

CHUNK = 64
Q_BLOCK = 128
ROPE_THETA = 10000.0
RMS_EPS = 1e-6

A_HEADS = D_MODEL // 128
A_HEAD_DIM = 64
D_A = A_HEADS * A_HEAD_DIM
IDX_HEADS = 8
IDX_DIM = 32
TOPK_MAX = 256

B_HEADS = D_MODEL // 256
B_HEAD_DIM = 64
D_B = B_HEADS * 2 * B_HEAD_DIM

D_MIX = D_A + D_B
D_FF = -(-8 * D_MODEL // (3 * 256)) * 256

IN_SPLITS = (D_A, D_A, D_A, IDX_HEADS * IDX_DIM, IDX_DIM, IDX_HEADS, D_B, D_B, D_B)
D_IN = D_A * 3 + IDX_HEADS * IDX_DIM + IDX_DIM + IDX_HEADS + D_B * 3

kernel_name = "hybrid_dsa_diffattn_swiglu_trunk"


def rms_norm(x, g):
    x32 = x.astype(jnp.float32)
    r = lax.rsqrt(jnp.mean(x32 * x32, axis=-1, keepdims=True) + RMS_EPS)
    return (x32 * r * g.astype(jnp.float32)).astype(x.dtype)


def rope_tables(seq_len, dim):
    pos = jnp.arange(seq_len, dtype=jnp.float32)
    inv = ROPE_THETA ** (-jnp.arange(0, dim, 2, dtype=jnp.float32) / dim)
    ang = pos[:, None] * inv[None, :]
    return jnp.cos(ang), jnp.sin(ang)


def apply_rope(x, cos, sin):
    half = x.shape[-1] // 2
    x32 = x.astype(jnp.float32)
    x1, x2 = x32[..., :half], x32[..., half:]
    c = cos[None, :, None, :]
    s = sin[None, :, None, :]
    return jnp.concatenate([x1 * c - x2 * s, x2 * c + x1 * s], axis=-1).astype(x.dtype)


def to_blocks(a):
    b, s = a.shape[:2]
    return jnp.swapaxes(a.reshape(b, s // Q_BLOCK, Q_BLOCK, *a.shape[2:]), 0, 1)


def from_blocks(a):
    nb, b = a.shape[:2]
    return jnp.swapaxes(a, 0, 1).reshape(b, nb * a.shape[2], *a.shape[3:])


def dsa_mixer(q, k, v, iq, ik, iw):
    s_len = q.shape[1]
    topk = min(TOPK_MAX, s_len // 4)
    key_chunk = jnp.arange(s_len) // CHUNK
    scale = A_HEAD_DIM ** -0.5
    gather = jax.vmap(lambda a, i: a[i])

    def block(args):
        bi, qb, iqb, iwb = args
        q_chunk = (bi * Q_BLOCK + jnp.arange(Q_BLOCK)) // CHUNK
        admissible = key_chunk[None, :] <= q_chunk[:, None]
        dots = jnp.einsum('bqhd,bsd->bqhs', iqb, ik,
                          preferred_element_type=jnp.float32) * (IDX_DIM ** -0.5)
        w = iwb.astype(jnp.float32) * (IDX_HEADS ** -0.5)
        score = jnp.einsum('bqhs,bqh->bqs', jax.nn.relu(dots), w)
        score = jnp.where(admissible[None], score, -jnp.inf)
        _, idx = lax.top_k(score, topk)
        k_sel = gather(k, idx)
        v_sel = gather(v, idx)
        valid = (idx // CHUNK) <= q_chunk[None, :, None]
        logits = jnp.einsum('bqhd,bqkhd->bhqk', qb, k_sel,
                            preferred_element_type=jnp.float32) * scale
        logits = jnp.where(valid[:, None], logits, -jnp.inf)
        p = jax.nn.softmax(logits, axis=-1)
        return jnp.einsum('bhqk,bqkhd->bqhd', p.astype(v.dtype), v_sel)

    nb = s_len // Q_BLOCK
    out = lax.map(block, (jnp.arange(nb), to_blocks(q), to_blocks(iq), to_blocks(iw)))
    return from_blocks(out)


def diff_mixer(q, k, v, lam, lam_init, g_subln):
    s_len = q.shape[1]
    key_chunk = jnp.arange(s_len) // CHUNK
    scale = B_HEAD_DIM ** -0.5

    def block(args):
        bi, qb = args
        q_chunk = (bi * Q_BLOCK + jnp.arange(Q_BLOCK)) // CHUNK
        admissible = key_chunk[None, :] <= q_chunk[:, None]
        logits = jnp.einsum('bqhcd,bshcd->bchqs', qb, k,
                            preferred_element_type=jnp.float32) * scale
        logits = jnp.where(admissible[None, None, None], logits, -jnp.inf)
        p = jax.nn.softmax(logits, axis=-1)
        attn = p[:, 0] - lam * p[:, 1]
        return jnp.einsum('bhqs,bshe->bqhe', attn.astype(v.dtype), v)

    nb = s_len // Q_BLOCK
    out = from_blocks(lax.map(block, (jnp.arange(nb), to_blocks(q))))
    return rms_norm(out, g_subln) * (1.0 - lam_init)


def setup_inputs(seed: int = 0) -> dict:
    key = jax.random.key(seed)
    ks = jax.random.split(key, 14)
    f32 = jnp.float32
    nrm = lambda k, shape, fan_in: jax.random.normal(k, shape, f32) * (fan_in ** -0.5)
    gain = lambda k, shape: 1.0 + 0.02 * jax.random.normal(k, shape, f32)
    return {
        "x": jax.random.normal(ks[0], (BATCH, SEQ, D_MODEL), f32),
        "w_in": nrm(ks[1], (DEPTH, D_MODEL, D_IN), D_MODEL),
        "w_out": nrm(ks[2], (DEPTH, D_MIX, D_MODEL), D_MIX),
        "g_mix": gain(ks[3], (DEPTH, D_MODEL)),
        "lam_q1": 0.1 * jax.random.normal(ks[4], (DEPTH, B_HEAD_DIM), f32),
        "lam_k1": 0.1 * jax.random.normal(ks[5], (DEPTH, B_HEAD_DIM), f32),
        "lam_q2": 0.1 * jax.random.normal(ks[6], (DEPTH, B_HEAD_DIM), f32),
        "lam_k2": 0.1 * jax.random.normal(ks[7], (DEPTH, B_HEAD_DIM), f32),
        "g_subln": gain(ks[8], (DEPTH, 2 * B_HEAD_DIM)),
        "g_ffn": gain(ks[9], (DEPTH, D_MODEL)),
        "w_gate": nrm(ks[10], (DEPTH, D_MODEL, D_FF), D_MODEL),
        "w_up": nrm(ks[11], (DEPTH, D_MODEL, D_FF), D_MODEL),
        "w_down": nrm(ks[12], (DEPTH, D_FF, D_MODEL), D_FF),
        "g_final": gain(ks[13], (D_MODEL,)),
    }


def reference(x, w_in, w_out, g_mix, lam_q1, lam_k1, lam_q2, lam_k2, g_subln,
              g_ffn, w_gate, w_up, w_down, g_final):
    b_, s_len, _ = x.shape
    cos_a, sin_a = rope_tables(s_len, A_HEAD_DIM)
    cos_i, sin_i = rope_tables(s_len, IDX_DIM)
    cos_b, sin_b = rope_tables(s_len, B_HEAD_DIM)
    split_points = [int(o) for o in np.cumsum(IN_SPLITS)[:-1]]

    for layer in range(DEPTH):
        h = rms_norm(x, g_mix[layer])
        proj = jnp.einsum('bsd,dc->bsc', h, w_in[layer])
        qa, ka, va, iq, ik, iw, qb, kb, vb = jnp.split(proj, split_points, axis=-1)

        qa = apply_rope(qa.reshape(b_, s_len, A_HEADS, A_HEAD_DIM), cos_a, sin_a)
        ka = apply_rope(ka.reshape(b_, s_len, A_HEADS, A_HEAD_DIM), cos_a, sin_a)
        va = va.reshape(b_, s_len, A_HEADS, A_HEAD_DIM)
        iq = apply_rope(iq.reshape(b_, s_len, IDX_HEADS, IDX_DIM), cos_i, sin_i)
        ik = apply_rope(ik[:, :, None, :], cos_i, sin_i)[:, :, 0, :]
        out_a = dsa_mixer(qa, ka, va, iq, ik, iw).reshape(b_, s_len, D_A)

        qb = apply_rope(qb.reshape(b_, s_len, 2 * B_HEADS, B_HEAD_DIM), cos_b, sin_b)
        kb = apply_rope(kb.reshape(b_, s_len, 2 * B_HEADS, B_HEAD_DIM), cos_b, sin_b)
        qb = qb.reshape(b_, s_len, B_HEADS, 2, B_HEAD_DIM)
        kb = kb.reshape(b_, s_len, B_HEADS, 2, B_HEAD_DIM)
        vb = vb.reshape(b_, s_len, B_HEADS, 2 * B_HEAD_DIM)
        lam_init = 0.8 - 0.6 * math.exp(-0.3 * layer)
        lam = (jnp.exp(jnp.sum(lam_q1[layer].astype(jnp.float32) * lam_k1[layer].astype(jnp.float32)))
               - jnp.exp(jnp.sum(lam_q2[layer].astype(jnp.float32) * lam_k2[layer].astype(jnp.float32)))
               + lam_init)
        out_b = diff_mixer(qb, kb, vb, lam, lam_init, g_subln[layer]).reshape(b_, s_len, D_B)

        mixed = jnp.concatenate([out_a, out_b], axis=-1)
        x = x + jnp.einsum('bsc,cd->bsd', mixed, w_out[layer])

        h2 = rms_norm(x, g_ffn[layer])
        gate = jnp.einsum('bsd,df->bsf', h2, w_gate[layer])
        up = jnp.einsum('bsd,df->bsf', h2, w_up[layer])
        x = x + jnp.einsum('bsf,fd->bsd', jax.nn.silu(gate) * up, w_down[layer])

    return rms_norm(x, g_final)
```

```python
import bisect
import contextlib
import math
import sys
import numpy as np
import ml_dtypes
import concourse.bass as bass
import concourse.mybir as mybir
from concourse.bass_utils import run_bass_kernel_spmd

F32 = mybir.dt.float32
BF = mybir.dt.bfloat16
AF = mybir.ActivationFunctionType
ALU = mybir.AluOpType

D = 1024
DIN = 3368
DFF = 2816
DEPTH = 2
EPS = 1e-6
NBIS = 16
NEG = -30000.0
ENGS = ['pe', 'act', 'dve', 'pool', 'sp']


class Op:
    pass


class _Stop(Exception):
    pass


STOP = None
INSMAP = None
FULLSYNC = True
P3MODE = 3
NAMES = {}


class Rec:
    def __getattr__(self, name):
        def f(*a, **k):
            self.call = (name, a, k)
            return self
        return f


class Prog:
    def __init__(self):
        self.ops = []
        self.lastw = {}
        self.rd = {}
        self.bar = []
        self.since_dma = []
        self.last_eng = {}

    def add(self, eng, fn, reads=(), writes=(), dma=None, cc=False):
        if getattr(self, 'stopped', False):
            return None
        o = Op()
        rec = Rec(); fn(rec)
        o.eng = eng; o.fn = rec.call; o.dma = dma; o.cc = cc
        o.deps = {}; o.needed = False; o.id = len(self.ops); o.sig = None
        o.line = sys._getframe(1).f_lineno

        def dep(d, kind):
            if d is None or d is o:
                return
            if d.dma is None and o.dma is None and d.eng == eng:
                if eng == 'pe' or (kind == 'war' and not FULLSYNC):
                    return
            o.deps[d.id] = d

        for r in reads:
            dep(self.lastw.get(r), 'raw')
        for r in writes:
            dep(self.lastw.get(r), 'waw')
            for q in self.rd.get(r, ()):
                dep(q, 'war')
        for b in self.bar:
            dep(b, 'war')
        for r in reads:
            self.rd.setdefault(r, []).append(o)
        for r in writes:
            self.lastw[r] = o
            self.rd[r] = []
        for d in o.deps.values():
            d.needed = True
        self.ops.append(o)
        if dma is not None:
            self.since_dma.append(o)
        else:
            self.last_eng[eng] = o
        return o

    def barrier(self):
        self.bar = list(self.last_eng.values()) + list(self.since_dma)
        self.since_dma = []
        self.nbar = getattr(self, 'nbar', 0) + 1
        if STOP is not None and self.nbar > STOP:
            self.stopped = True

    def emit(self, nc, stack):
        chans = sorted({o.dma for o in self.ops if o.dma is not None and not o.cc})
        esem = {e: stack.enter_context(nc.semaphore("e_" + e)) for e in ENGS}
        csem = {c: stack.enter_context(nc.semaphore("c_" + c)) for c in chans}
        cnt = {e: 0 for e in ENGS}
        chn = {c: 0 for c in chans}
        for o in self.ops:
            if o.cc:
                o.sig = (stack.enter_context(nc.semaphore("cc%d" % o.id)), 1)
            elif o.dma is not None:
                chn[o.dma] += 1
                o.sig = (csem[o.dma], 16 * chn[o.dma])
            elif o.needed:
                cnt[o.eng] += 1
                o.sig = (esem[o.eng], cnt[o.eng])
        per = {e: [o for o in self.ops if o.eng == e] for e in ENGS}
        chan_ids = {c: [o.id for o in self.ops if o.dma == c and not o.cc] for c in chans}
        block = stack.enter_context(nc.Block())

        def run(e, eng):
            waited = {}
            for o in per[e]:
                need = {}
                for d in o.deps.values():
                    sem, val = d.sig
                    if d.dma is not None and not d.cc:
                        val = 16 * bisect.bisect_left(chan_ids[d.dma], o.id)
                    k = id(sem)
                    if need.get(k, (None, 0))[1] < val:
                        need[k] = (sem, val)
                for k, (sem, val) in need.items():
                    if waited.get(k, 0) < val:
                        eng.wait_ge(sem, val)
                        waited[k] = val
                nm, a_, k_ = o.fn
                ins = getattr(eng, nm)(*a_, **k_)
                if INSMAP is not None:
                    try:
                        INSMAP[str(ins.ins.name)] = o.line
                    except Exception:
                        pass
                if o.cc:
                    ins.then_inc(o.sig[0])
                elif o.dma is not None:
                    ins.then_inc(o.sig[0], 16)
                elif o.needed:
                    ins.then_inc(o.sig[0], 1)
            if e == 'sp':
                mx = {}
                for o in self.ops:
                    if o.sig is not None:
                        sem, val = o.sig
                        if mx.get(id(sem), (None, 0))[1] < val:
                            mx[id(sem)] = (sem, val)
                for k, (sem, val) in mx.items():
                    if waited.get(k, 0) < val:
                        eng.wait_ge(sem, val)

        block.tensor(lambda eng: run('pe', eng))
        block.scalar(lambda eng: run('act', eng))
        block.vector(lambda eng: run('dve', eng))
        block.gpsimd(lambda eng: run('pool', eng))
        block.sync(lambda eng: run('sp', eng))


def build(S):
    TPC = S // 2
    NT = TPC // 128
    A8 = TPC // 512
    nc = bass.Bass("TRN2", target_bir_lowering=False)
    P = Prog()

    def din(name, shape, dt=F32):
        return nc.dram_tensor(name, list(shape), dt, kind="ExternalInput").ap()

    x_in = din("x", [TPC, D])
    w_in = din("w_in", [DEPTH, D, DIN])
    w_out = din("w_out", [DEPTH, D, D])
    w_gate = din("w_gate", [DEPTH, D, DFF])
    w_up = din("w_up", [DEPTH, D, DFF])
    w_down = din("w_down", [DEPTH, DFF, D])
    g_mix = din("g_mix", [DEPTH, D])
    g_ffn = din("g_ffn", [DEPTH, D])
    g_final = din("g_final", [1, D])
    g_subln = din("g_subln", [DEPTH, 128])
    lamv = din("lamv", [DEPTH, 4, 64])
    rope = din("rope", [TPC, 96])
    ident_in = din("ident", [128, 128], BF)
    cbf_in = din("cbf", [128, 256])
    cbb_in = din("cbb", [128, 256], BF)
    pow2_in = din("pow2", [128, 32])
    y_out = nc.dram_tensor("y", [TPC, D], F32, kind="ExternalOutput").ap()

    def dscr(name, shape, dt=BF):
        return nc.dram_tensor(name, list(shape), dt)

    xres = dscr("xres", [TPC, D], F32).ap()
    mixed = dscr("mixed", [TPC, D], BF).ap()
    qaT = dscr("qaT", [512, TPC]).ap().rearrange("(c p) t -> p c t", p=128)
    qbT = dscr("qbT", [512, TPC]).ap().rearrange("(c p) t -> p c t", p=128)
    iqT = dscr("iqT", [384, TPC]).ap().rearrange("(c p) t -> p c t", p=128)
    mbs = dscr("mbs", [NT, 128, S]).ap()
    UR = 256 * TPC // 512
    kT_loc = [dscr("kTl%d" % u, [UR, 512]) for u in range(4)]
    kT_all = [dscr("kTa%d" % u, [2 * UR, 512]) for u in range(4)]
    v_loc = [dscr("vl%d" % u, [TPC // 2, 512]) for u in range(4)]
    v_all = [dscr("va%d" % u, [TPC, 512]) for u in range(4)]
    IR = max(32 * TPC // 512, 32)
    ik_loc = dscr("ikl", [IR, 512])
    ik_all = dscr("ika", [2 * IR, 512])

    def kT_loc_view(u):
        return kT_loc[u].ap().rearrange("(c p a) b -> p c (a b)", p=128, a=A8)

    def kT_all_view(u):
        return kT_all[u].ap().rearrange("(r c p a) b -> r p c (a b)", r=2, p=128, a=A8)

    def v_all_view(u):
        return v_all[u].ap().rearrange("(r t) b -> r t b", r=2)

    ik_loc_v = ik_loc.ap().rearrange("(p a) b -> p (a b)", a=A8)
    ik_all_v = ik_all.ap().rearrange("(r p a) b -> r p (a b)", r=2, a=A8)

    stack = contextlib.ExitStack()
    with stack:
        uid = [0]

        def un(name):
            uid[0] += 1
            NAMES.setdefault(name, []).append("s%d_%s" % (uid[0], name))
            return "s%d_%s" % (uid[0], name)

        def sb(name, shape, dt):
            return stack.enter_context(nc.sbuf_tensor(un(name), list(shape), dt))

        ps = stack.enter_context(nc.psum_tensor("ps", [128, 8, 512], F32))

        def psb(b0, nb=1):
            return ps[:, b0, :].bitcast(BF)

        ident = sb("ident", [128, 128], BF)
        ident4 = sb("ident4", [128, 512], BF)
        cbf = sb("cbf", [128, 256], F32)
        cbb = sb("cbb", [128, 256], BF)
        pow2 = sb("pow2", [128, 32], F32)
        mhalf = sb("mhalf", [128, 64], F32)
        ssA = sb("ssA", [128, NT], F32)
        rstd = sb("rstd", [128, NT], F32)
        iw_all = sb("iw_all", [128, NT, 8], F32)
        lam_t = sb("lam_t", [128, 4, 64], F32)
        lam_s = sb("lam_s", [128, 8], F32)
        gsub = sb("gsub", [128, 128], F32)
        junk = sb("junk", [128, 1024], BF)
        P.add('sp', lambda e: e.dma_start(out=ident[:], in_=ident_in), (), ['ident'], dma='c0')
        for q_ in range(4):
            P.add('pool', lambda e, q_=q_: e.tensor_copy(out=ident4[:, q_ * 128:(q_ + 1) * 128], in_=ident[:]), ['ident'], ['ident4'])
        P.add('sp', lambda e: e.dma_start(out=cbf[:], in_=cbf_in), (), ['cbf'], dma='c1')
        P.add('sp', lambda e: e.dma_start(out=cbb[:], in_=cbb_in), (), ['cbb'], dma='c2')
        P.add('sp', lambda e: e.dma_start(out=pow2[:], in_=pow2_in), (), ['pow2'], dma='c3')
        P.add('pool', lambda e: e.memset(mhalf[:], -0.5), (), ['mhalf'])

        def load_weight(dst, src_rows, ncols, wst, tag, chunk=842):
            c0 = 0
            k = load_weight.k
            while c0 < ncols:
                n = min(chunk, ncols - c0)
                s = k % 2
                P.add('sp', lambda e, s=s, c0=c0, n=n: e.dma_start(out=wst[:, s, 0:n], in_=src_rows[:, c0:c0 + n]),
                      (), [('wst', s)], dma='wst%d' % s)
                P.add('pool', lambda e, s=s, c0=c0, n=n: e.tensor_copy(out=dst[:, c0:c0 + n], in_=wst[:, s, 0:n]),
                      [('wst', s)], [tag])
                c0 += n
                k += 1
            load_weight.k = k
        load_weight.k = 0

        def rstd_from_ss(ss_ap, rstd_ap, n, inv):
            P.add('dve', lambda e: e.tensor_scalar(out=ss_ap, in0=ss_ap, scalar1=inv, scalar2=EPS,
                                                   op0=ALU.mult, op1=ALU.add), ['ss'], ['ss'])
            P.add('pool', lambda e: e.tensor_tensor(out=rstd_ap, in0=ss_ap, in1=mhalf[:, 0:n], op=ALU.pow),
                  ['ss', 'mhalf'], ['rstd'])

        with contextlib.ExitStack() as st0:
            xt = st0.enter_context(nc.sbuf_tensor(un("xt_s"), [128, 2, D], F32))
            for it in range(NT):
                s = it % 2
                P.add('sp', lambda e, s=s, it=it: e.dma_start(out=xt[:, s, :], in_=x_in[it * 128:(it + 1) * 128, :]),
                      (), [('xt', s)], dma='xt%d' % s)
                P.add('act', lambda e, s=s, it=it: e.activation(out=junk[:, :], in_=xt[:, s, :], func=AF.Square,
                                                                accum_out=ssA[:, it:it + 1]),
                      [('xt', s)], ['ss', 'junk'])
            rstd_from_ss(ssA[:, :], rstd[:, :], NT, 1.0 / D)
            P.barrier()

        try:
            for L in range(DEPTH if STOP is None or STOP > 0 else 0):
                xsrc = x_in if L == 0 else xres
                lam_init = 0.8 - 0.6 * math.exp(-0.3 * L)
                P.add('sp', lambda e, L=L: e.dma_start(out=lam_t[:].rearrange("p a b -> p (a b)"),
                                                       in_=lamv[L:L + 1].rearrange("o a b -> o (a b)").partition_broadcast(128)),
                      (), ['lam_t'], dma='c0')
                P.add('sp', lambda e, L=L: e.dma_start(out=gsub[:], in_=g_subln[L:L + 1, :].partition_broadcast(128)),
                      (), ['gsub'], dma='c1')
                for j in range(2):
                    P.add('dve', lambda e, j=j: e.scalar_tensor_tensor(out=junk[:, 0:64], in0=lam_t[:, 2 * j, :], scalar=1.0,
                                                                      in1=lam_t[:, 2 * j + 1, :], op0=ALU.mult, op1=ALU.mult,
                                                                      accum_out=lam_s[:, j:j + 1]),
                          ['lam_t'], ['junk', 'lam_s'])
                P.add('act', lambda e: e.activation(out=lam_s[:, 2:4], in_=lam_s[:, 0:2], func=AF.Exp), ['lam_s'], ['lam_e'])
                P.add('dve', lambda e: e.tensor_tensor(out=lam_s[:, 4:5], in0=lam_s[:, 3:4], in1=lam_s[:, 2:3], op=ALU.subtract),
                      ['lam_e'], ['lam_d'])
                P.add('dve', lambda e, li=lam_init: e.tensor_scalar(out=lam_s[:, 5:6], in0=lam_s[:, 4:5], scalar1=-li, scalar2=None,
                                                                    op0=ALU.add), ['lam_d'], ['neglam'])
                P.add('dve', lambda e, li=lam_init: e.tensor_scalar(out=gsub[:], in0=gsub[:], scalar1=1.0 - li, scalar2=None,
                                                                    op0=ALU.mult), ['gsub'], ['gsub'])
                neglam = lam_s[:, 5:6]

                with contextlib.ExitStack() as st1:
                    def sb1(name, shape, dt):
                        return st1.enter_context(nc.sbuf_tensor(un(name), list(shape), dt))
                    wbf = sb1("wbf", [128, 8, DIN], BF)
                    wst = sb1("wst1", [128, 2, 842], F32)
                    gbc = sb1("gbc", [128, D], F32)
                    xt = sb1("xt", [128, 2, D], F32)
                    rt = sb1("rt", [128, 2, 96], F32)
                    hb = sb1("hb", [128, 2, D], BF)
                    hT = sb1("hT", [128, 2, 8, 128], BF)
                    pr = sb1("pr", [128, 2, DIN], F32)
                    prb = sb1("prb", [128, 2, DIN], BF)
                    tqs = sb1("tqs", [128, 2, 24, 128], BF)
                    tA = sb1("tA", [128, 2, 16, 32], F32)
                    tB = sb1("tB", [128, 2, 16, 32], F32)
                    for s_ in range(2):
                        P.add('pool', lambda e, s_=s_: e.memset(tqs[:, s_, 8:12, :].rearrange("p a b -> p (a b)"), 0.0), (),
                              [('tqs', s_, 1, 8), ('tqs', s_, 1, 10), ('tqs', s_, 1, 11)])
                    P.add('sp', lambda e, L=L: e.dma_start(out=gbc[:], in_=g_mix[L:L + 1, :].partition_broadcast(128)),
                          (), ['gbc'], dma='c2')
                    for kc in range(8):
                        load_weight(wbf[:, kc, :], w_in[L, kc * 128:(kc + 1) * 128, :], DIN, wst, 'wbf')
                    tch = [(c * 128, 128) for c in range(8)]
                    tch += [(1536, 96), (1632, 96), (1728, 64)]
                    tch += [(1792, 32)]
                    tch += [(1832 + c * 128, 128) for c in range(8)]
                    for it in range(NT):
                        s = it % 2
                        t0 = it * 128
                        P.add('sp', lambda e, s=s, t0=t0: e.dma_start(out=xt[:, s, :], in_=xsrc[t0:t0 + 128, :]),
                              (), [('xt', s)], dma='xt%d' % s)
                        P.add('sp', lambda e, s=s, t0=t0: e.dma_start(out=rt[:, s, :], in_=rope[t0:t0 + 128, :]),
                              (), [('rt', s)], dma='rt%d' % s)
                        P.add('dve', lambda e, s=s, it=it: e.scalar_tensor_tensor(out=hb[:, s, :], in0=xt[:, s, :],
                                                                                   scalar=rstd[:, it:it + 1], in1=gbc[:],
                                                                                   op0=ALU.mult, op1=ALU.mult),
                              [('xt', s), 'rstd', 'gbc'], [('hb', s)])
                        for kc in range(8):
                            P.add('pe', lambda e, s=s, kc=kc: e.transpose(out=psb(0)[:, kc * 128:(kc + 1) * 128],
                                                                          in_=hb[:, s, kc * 128:(kc + 1) * 128], identity=ident[:]),
                                  [('hb', s), 'ident'], ['pb0'])
                        P.add('act', lambda e, s=s: e.copy(out=hT[:, s].rearrange("p a b -> p (a b)"), in_=psb(0)[:, :]),
                              ['pb0'], [('hT', s)])
                        ncg = (DIN + 511) // 512
                        for cg in range(ncg):
                            c0 = cg * 512
                            n = min(512, DIN - c0)
                            b = 1 + cg % 2
                            for kc in range(8):
                                P.add('pe', lambda e, s=s, kc=kc, c0=c0, n=n, b=b: e.matmul(
                                    ps[:, b, 0:n], lhsT=hT[:, s, kc, :], rhs=wbf[:, kc, c0:c0 + n],
                                    start=(kc == 0), stop=(kc == 7)), [('hT', s), 'wbf'], [('pb', b)])
                            if cg % 2 == 0:
                                P.add('act', lambda e, s=s, c0=c0, n=n, b=b: e.copy(out=pr[:, s, c0:c0 + n], in_=ps[:, b, 0:n]),
                                      [('pb', b)], [('pr', s, cg)])
                            else:
                                P.add('dve', lambda e, s=s, c0=c0, n=n, b=b: e.tensor_copy(out=pr[:, s, c0:c0 + n], in_=ps[:, b, 0:n]),
                                      [('pb', b)], [('pr', s, cg)])
                        def rope_grp(eng, tmp, c0, H, half, coff, soff, s=s):
                            xv = pr[:, s, c0:c0 + H * 2 * half].rearrange("p (h j d) -> p h j d", h=H, j=2)
                            ov = prb[:, s, c0:c0 + H * 2 * half].rearrange("p (h j d) -> p h j d", h=H, j=2)
                            cosb = rt[:, s, coff:coff + half].unsqueeze(1).to_broadcast([128, H, half])
                            sinb = rt[:, s, soff:soff + half].unsqueeze(1).to_broadcast([128, H, half])
                            t1 = tmp[:, 0, 0:H, 0:half]
                            t2 = tmp[:, 1, 0:H, 0:half]
                            rs_ = [('pr', s, q_) for q_ in range(7)] + [('rt', s)]
                            tg = ('tmp', eng)
                            P.add(eng, lambda e: e.tensor_tensor(out=t1, in0=xv[:, :, 0, :], in1=cosb, op=ALU.mult), rs_, [(tg, 0)])
                            P.add(eng, lambda e: e.tensor_tensor(out=t2, in0=xv[:, :, 1, :], in1=sinb, op=ALU.mult), rs_, [(tg, 1)])
                            P.add(eng, lambda e: e.tensor_tensor(out=ov[:, :, 0, :], in0=t1, in1=t2, op=ALU.subtract),
                                  [(tg, 0), (tg, 1)], [('prb', s, c0)])
                            P.add(eng, lambda e: e.tensor_tensor(out=t1, in0=xv[:, :, 1, :], in1=cosb, op=ALU.mult), rs_, [(tg, 0)])
                            P.add(eng, lambda e: e.tensor_tensor(out=t2, in0=xv[:, :, 0, :], in1=sinb, op=ALU.mult), rs_, [(tg, 1)])
                            P.add(eng, lambda e: e.tensor_tensor(out=ov[:, :, 1, :], in0=t1, in1=t2, op=ALU.add),
                                  [(tg, 0), (tg, 1)], [('prb', s, c0)])
                        rope_grp('dve', tA, 0, 16, 32, 0, 32)
                        rope_grp('pool', tB, 1832, 16, 32, 0, 32)
                        rope_grp('dve', tA, 1536, 9, 16, 64, 80)
                        P.add('pool', lambda e, s=s: e.tensor_copy(out=prb[:, s, 1024:1536], in_=pr[:, s, 1024:1536]),
                              [('pr', s, q_) for q_ in range(7)], [('prb', s, 'v')])
                        P.add('pool', lambda e, s=s: e.tensor_copy(out=prb[:, s, 2856:3368], in_=pr[:, s, 2856:3368]),
                              [('pr', s, q_) for q_ in range(7)], [('prb', s, 'v')])
                        P.add('pool', lambda e, s=s, it=it: e.tensor_copy(out=iw_all[:, it, :], in_=pr[:, s, 1824:1832]),
                              [('pr', s, q_) for q_ in range(7)], ['iw_all'])
                        for bi in range(3):
                            lo, hi = bi * 8, min(bi * 8 + 8, len(tch))
                            for k in range(lo, hi):
                                c0, n = tch[k]
                                P.add('pe', lambda e, s=s, k=k, c0=c0, n=n, bi=bi: e.transpose(
                                    out=psb(3 + bi)[0:n, (k - bi * 8) * 128:(k - bi * 8 + 1) * 128],
                                    in_=prb[:, s, c0:c0 + n], identity=ident[:]), [('prb', s, 0), ('prb', s, 1832), ('prb', s, 1536), 'ident'], [('pb', 3 + bi)])
                            if bi == 1:
                                pieces = [(96, 8, 10), (64, 10, 11), (32, 11, 12), (128, 12, 16)]
                            else:
                                pieces = [(128, lo, hi)]
                            for (nr, a0, a1) in pieces:
                                o0, o1 = (a0 - lo) * 128, (a1 - lo) * 128
                                if bi % 2 == 0:
                                    P.add('act', lambda e, s=s, a0=a0, a1=a1, o0=o0, o1=o1, nr=nr, bi=bi: e.copy(
                                        out=tqs[0:nr, s, a0:a1, :].rearrange("p a b -> p (a b)"), in_=psb(3 + bi)[0:nr, o0:o1]),
                                        [('pb', 3 + bi)], [('tqs', s, bi, a0)])
                                else:
                                    P.add('dve', lambda e, s=s, a0=a0, a1=a1, o0=o0, o1=o1, nr=nr, bi=bi: e.tensor_copy(
                                        out=tqs[0:nr, s, a0:a1, :].rearrange("p a b -> p (a b)"), in_=psb(3 + bi)[0:nr, o0:o1]),
                                        [('pb', 3 + bi)], [('tqs', s, bi, a0)])
                        ch = 'st%d' % s
                        rd_all = [('tqs', s, 0, 0), ('tqs', s, 1, 8), ('tqs', s, 1, 10), ('tqs', s, 1, 11), ('tqs', s, 1, 12), ('tqs', s, 2, 16)]
                        P.add('act', lambda e, s=s, t0=t0: e.dma_start(out=qaT[:, :, t0:t0 + 128], in_=tqs[:, s, 0:4, :]),
                              rd_all, ['d_qaT'], dma=ch)
                        for u in range(2):
                            P.add('act', lambda e, s=s, t0=t0, u=u: e.dma_start(out=kT_loc_view(u)[:, :, t0:t0 + 128],
                                                                                in_=tqs[:, s, 4 + 2 * u:6 + 2 * u, :]),
                                  rd_all, ['d_kT'], dma=ch)
                        P.add('act', lambda e, s=s, t0=t0: e.dma_start(out=iqT[0:96, :, t0:t0 + 128], in_=tqs[0:96, s, 8:11, :]),
                              rd_all, ['d_iqT'], dma=ch)
                        P.add('act', lambda e, s=s, t0=t0: e.dma_start(out=ik_loc_v[:, t0:t0 + 128], in_=tqs[0:32, s, 11, :]),
                              rd_all, ['d_ik'], dma=ch)
                        P.add('act', lambda e, s=s, t0=t0: e.dma_start(out=qbT[:, :, t0:t0 + 128], in_=tqs[:, s, 12:16, :]),
                              rd_all, ['d_qbT'], dma=ch)
                        for u in range(2):
                            P.add('act', lambda e, s=s, t0=t0, u=u: e.dma_start(out=kT_loc_view(2 + u)[:, :, t0:t0 + 128],
                                                                                in_=tqs[:, s, 16 + 2 * u:18 + 2 * u, :]),
                                  rd_all, ['d_kT'], dma=ch)
                        hu = it // (NT // 2)
                        tl = (it % (NT // 2)) * 128
                        P.add('pool', lambda e, s=s, hu=hu, tl=tl: e.dma_start(out=v_loc[hu].ap()[tl:tl + 128, :],
                                                                               in_=prb[:, s, 1024:1536]),
                              [('prb', s, 'v')], ['d_v'], dma='sv%d' % s)
                        P.add('pool', lambda e, s=s, hu=hu, tl=tl: e.dma_start(out=v_loc[2 + hu].ap()[tl:tl + 128, :],
                                                                               in_=prb[:, s, 2856:3368]),
                              [('prb', s, 'v')], ['d_v'], dma='sv%d' % s)
                    P.barrier()

                groups = [[0, 1], [2, 3], [4, 5], [6, 7]]
                for a, b in [(kT_loc[u], kT_all[u]) for u in range(4)] + [(v_loc[u], v_all[u]) for u in range(4)] + [(ik_loc, ik_all)]:
                    P.add('pool', lambda e, a=a, b=b: e.collective_compute("AllGather", ALU.bypass, replica_groups=groups,
                                                                           ins=[a.ap().opt()], outs=[b.ap().opt()]),
                          (), ['gath'], dma='cc', cc=True)
                P.barrier()

                with contextlib.ExitStack() as st3:
                    def sb3(name, shape, dt):
                        return st3.enter_context(nc.sbuf_tensor(un(name), list(shape), dt))
                    ikr = sb3("ikr", [96, 3, S], BF)
                    iqt = sb3("iqt", [96, 2, 3, 128], BF)
                    Rt = sb3("Rt", [128, 8, 256], BF)
                    Dg = sb3("Dg", [128, 2, 8, 128], BF)
                    Ssb = sb3("Ssb", [128, S], F32)
                    cjunk = sb3("cjunk", [128, S], BF)
                    mbt = sb3("mbt", [128, 2, S], BF)
                    sm = sb3("sm", [128, 16], F32)
                    W2 = sb3("W2", [128, 32], F32)
                    P.add('pool', lambda e: e.memset(ikr[:].rearrange("p g k -> p (g k)"), 0.0), (), ['ikr0'])
                    ikv = ikr[:].rearrange("p g (l r k) -> p g l r k", r=2, k=128)
                    for g in range(3):
                        for r in range(2):
                            P.add('sp', lambda e, g=g, r=r: e.dma_start(
                                out=ikv[32 * g:32 * g + 32, g, :, r, :],
                                in_=ik_all_v[r].rearrange("p (l k) -> p l k", k=128)), ['ikr0'], ['ikr'], dma='ikr')
                    for i in range(NT):
                        s = i % 2
                        N = (2 * i + 2) * 128
                        t0 = i * 128
                        P.add('sp', lambda e, s=s, t0=t0: e.dma_start(out=iqt[:, s], in_=iqT[0:96, :, t0:t0 + 128]),
                              (), [('iqt', s)], dma='iqt%d' % s)
                        for h in range(8):
                            P.add('pool', lambda e, s=s, i=i, h=h: e.tensor_scalar(
                                out=Dg[:, s, h, :], in0=ident[:], scalar1=iw_all[:, i, h:h + 1], scalar2=None, op0=ALU.mult),
                                ['ident', 'iw_all'], [('Dg', s)])
                        for kg in range(N // 256):
                            k0 = kg * 256
                            for h in range(8):
                                c, g = h // 3, h % 3
                                P.add('pe', lambda e, s=s, h=h, c=c, g=g, k0=k0: e.matmul(
                                    ps[:, h // 2, (h % 2) * 256:(h % 2) * 256 + 256],
                                    lhsT=iqt[0:96, s, c, :], rhs=ikr[0:96, g, k0:k0 + 256],
                                    start=True, stop=True), [('iqt', s), 'ikr'], [('dots', h // 4)])
                            P.add('act', lambda e: e.activation(out=Rt[:, 0:4, :].rearrange("p a b -> p (a b)"),
                                                                in_=ps[:, 0:2, :].rearrange("p a b -> p (a b)"), func=AF.Relu),
                                  [('dots', 0)], [('Rt', 0)])
                            P.add('dve', lambda e: e.tensor_scalar(out=Rt[:, 4:8, :].rearrange("p a b -> p (a b)"),
                                                                   in0=ps[:, 2:4, :].rearrange("p a b -> p (a b)"),
                                                                   scalar1=0.0, scalar2=None, op0=ALU.max),
                                  [('dots', 1)], [('Rt', 1)])
                            sbk = 4 + kg % 2
                            for h in range(8):
                                P.add('pe', lambda e, s=s, h=h, sbk=sbk: e.matmul(
                                    ps[:, sbk, 0:256], lhsT=Dg[:, s, h, :], rhs=Rt[:, h, :],
                                    start=(h == 0), stop=(h == 7)), [('Dg', s), ('Rt', h // 4)], [('pb', sbk)])
                            if kg % 2 == 0:
                                P.add('act', lambda e, sbk=sbk, k0=k0: e.copy(out=Ssb[:, k0:k0 + 256], in_=ps[:, sbk, 0:256]),
                                      [('pb', sbk)], ['Ssb'])
                            else:
                                P.add('dve', lambda e, sbk=sbk, k0=k0: e.tensor_copy(out=Ssb[:, k0:k0 + 256], in_=ps[:, sbk, 0:256]),
                                      [('pb', sbk)], ['Ssb'])
                        thr = sm[:, 0:1]
                        if i >= 1 and P3MODE >= 2:
                            P.add('dve', lambda e, N=N: e.tensor_reduce(out=sm[:, 1:2], in_=Ssb[:, 0:N], axis=mybir.AxisListType.X,
                                                                        op=ALU.max, apply_absolute_value=True),
                                  ['Ssb'], ['amax'])
                        P.add('dve', lambda e, N=N: e.tensor_tensor(out=Ssb[:, N - 256:N], in0=Ssb[:, N - 256:N], in1=cbf[:],
                                                                    op=ALU.add), ['Ssb', 'cbf'], ['Ssb'])
                        if i >= 1 and P3MODE >= 3:
                            P.add('dve', lambda e: e.tensor_scalar(out=W2[:, :], in0=pow2[:, :], scalar1=sm[:, 1:2], scalar2=None,
                                                                   op0=ALU.mult), ['amax', 'pow2'], ['W2'])
                            P.add('dve', lambda e: e.memset(sm[:, 2:3], 0.0), (), [('mid', 0)])
                            for k in range(NBIS):
                                a, b = 2 + (k % 2), 2 + ((k + 1) % 2)
                                P.add('dve', lambda e, N=N, a=a: e.tensor_scalar(
                                    out=cjunk[:, 0:N], in0=Ssb[:, 0:N], scalar1=sm[:, a:a + 1], scalar2=0.0,
                                    op0=ALU.is_ge, op1=ALU.add, accum_out=sm[:, 4:5]),
                                    ['Ssb', ('mid', k % 2)], ['cnt', 'cjunk'])
                                P.add('dve', lambda e: e.tensor_scalar(out=sm[:, 5:6], in0=sm[:, 4:5], scalar1=255.5, scalar2=0.5,
                                                                       op0=ALU.is_ge, op1=ALU.subtract), ['cnt'], ['dsg'])
                                P.add('dve', lambda e, a=a, b=b, k=k: e.scalar_tensor_tensor(
                                    out=sm[:, b:b + 1], in0=sm[:, 5:6], scalar=W2[:, k:k + 1], in1=sm[:, a:a + 1],
                                    op0=ALU.mult, op1=ALU.add), ['dsg', 'W2', ('mid', k % 2)], [('mid', (k + 1) % 2)])
                            fin = 2 + (NBIS % 2)
                            P.add('dve', lambda e, fin=fin: e.tensor_tensor(out=thr, in0=sm[:, fin:fin + 1],
                                                                            in1=W2[:, NBIS:NBIS + 1], op=ALU.subtract),
                                  [('mid', NBIS % 2), 'W2'], ['thr'])
                        else:
                            P.add('dve', lambda e: e.memset(thr, -1e29), (), ['thr'])
                        P.add('dve', lambda e, s=s, N=N: e.tensor_scalar(out=mbt[:, s, 0:N], in0=Ssb[:, 0:N], scalar1=thr,
                                                                        scalar2=NEG, op0=ALU.is_lt, op1=ALU.mult),
                              ['Ssb', 'thr'], [('mbt', s)])
                        P.add('sp', lambda e, s=s, i=i, N=N: e.dma_start(out=mbs[i, :, 0:N], in_=mbt[:, s, 0:N]),
                              [('mbt', s)], ['d_mb'], dma='smb%d' % s)
                    P.barrier()

                def attn_pass(kind, grp):
                    VW = 65 if kind == 'dsa' else 129
                    NV = 4 if kind == 'dsa' else 2
                    with contextlib.ExitStack() as st4:
                        def sb4(name, shape, dt):
                            return st4.enter_context(nc.sbuf_tensor(un(name), list(shape), dt))
                        kres = sb4("kres", [128, 2, S], BF)
                        vres = sb4("vres", [128, S // 128, NV, VW], BF)
                        qt = sb4("qt", [128, 2, 2, 2, 128], BF)
                        pT = sb4("pT", [128, 2, 8, 128], BF)
                        mix = sb4("mix", [128, 2, 256], BF)
                        fs = sb4("fs", [128, 2, 16], F32)
                        ftmp = sb4("ftmp", [128, 2, 128], F32)
                        mbt = sb4("mbt4", [128, 2, S], BF) if kind == 'dsa' else None
                        ubase = (0 if kind == 'dsa' else 2) + grp
                        qsrc = qaT if kind == 'dsa' else qbT
                        kview = kT_all_view(ubase)
                        kv5 = kres[:].rearrange("p c (l r k) -> p c l r k", r=2, k=128)
                        for r in range(2):
                            for c in range(2):
                                P.add('sp', lambda e, r=r, c=c: e.dma_start(
                                    out=kv5[:, c, :, r, :], in_=kview[r, :, c, :].rearrange("p (l k) -> p l k", k=128)),
                                    (), ['kres'], dma='kres')
                        P.add('pool', lambda e: e.memset(vres[:].rearrange("p a b c -> p (a b c)"), 1.0), (), ['vres'])
                        P.add('pool', lambda e: e.memset(qt[:].rearrange("p a b c d -> p (a b c d)"), 0.0), (), [('qt', 0, 0), ('qt', 0, 1), ('qt', 1, 0), ('qt', 1, 1)])
                        vsrc0 = 0 if kind == 'dsa' else 2
                        VLAST = 2 * (1 * (NT // 2) + (NT // 2 - 1)) + 1
                        c0 = grp * 256
                        for r in range(2):
                            for hu in range(2):
                                vv = v_all_view(vsrc0 + hu)[r]
                                nl = NT // 2
                                for l in range(nl):
                                    kt = 2 * (hu * nl + l) + r
                                    P.add('sp', lambda e, vv=vv, l=l, kt=kt: e.dma_start(
                                        out=vres[:, kt, :, 0:VW - 1],
                                        in_=vv[l * 128:(l + 1) * 128, c0:c0 + 256].rearrange("p (a b) -> p a b", a=NV)),
                                        ['vres'], [('vres', kt)], dma='vres')
                        for i in range(NT):
                            s = i % 2
                            t0 = i * 128
                            NK = 2 * i + 2
                            for e2_ in range(2):
                                P.add('sp', lambda e, s=s, t0=t0, e2_=e2_: e.dma_start(
                                    out=qt[64 * e2_:64 * e2_ + 64, s, :, e2_, :],
                                    in_=qsrc[64 * e2_:64 * e2_ + 64, 2 * grp:2 * grp + 2, t0:t0 + 128]),
                                    (), [('qt', s, e2_)], dma='qt%d' % s)
                            if kind == 'dsa':
                                P.add('sp', lambda e, s=s, i=i, NK=NK: e.dma_start(out=mbt[:, s, 0:NK * 128], in_=mbs[i, :, 0:NK * 128]),
                                      (), [('mbt', s)], dma='mbt%d' % s)
                            accb = 4 + 2 * s
                            NP = NK // 2

                            def rec_logits(kp, s=s, NP=NP):
                                ls = kp % 2
                                lb = 2 * ls
                                last = (kp == NP - 1)
                                for kl in range(2):
                                    kt = 2 * kp + kl
                                    need_mask = (kind == 'dsa') or last
                                    if need_mask:
                                        if kind == 'dsa':
                                            ml = mbt[:, s, kt * 128:(kt + 1) * 128]
                                            rr = [('mbt', s), 'ident4']
                                        else:
                                            ml = cbb[:, kl * 128:(kl + 1) * 128]
                                            rr = ['cbb', 'ident4']
                                        P.add('pe', lambda e, ml=ml, kl=kl, lb=lb: e.matmul(
                                            ps[:, lb + kl, :], lhsT=ml, rhs=ident4[:],
                                            start=True, stop=False, skip_group_check=True), rr, [('lg', ls)])
                                    for u in range(4):
                                        c, e2 = u // 2, u % 2
                                        P.add('pe', lambda e, c=c, e2=e2, kt=kt, kl=kl, u=u, lb=lb, need_mask=need_mask: e.matmul(
                                            ps[:, lb + kl, u * 128:(u + 1) * 128],
                                            lhsT=kres[:, c, kt * 128:(kt + 1) * 128],
                                            rhs=qt[:, s, c, e2, :], start=(not need_mask), stop=True, skip_group_check=True),
                                            ['kres', ('qt', s, 0), ('qt', s, 1)], [('lg', ls)])
                                P.add('act', lambda e, ls=ls, lb=lb: e.activation(
                                    out=pT[:, ls].rearrange("p a b -> p (a b)"),
                                    in_=ps[:, lb:lb + 2, :].rearrange("p a b -> p (a b)"), func=AF.Exp, scale=0.125),
                                    [('lg', ls)], [('pT', ls)])

                            def rec_av(kp, s=s, NK=NK, accb=accb):
                                ls = kp % 2
                                for kl in range(2):
                                    kt = 2 * kp + kl
                                    for u in range(4):
                                        vi = u if kind == 'dsa' else u // 2
                                        P.add('pe', lambda e, ls=ls, kl=kl, u=u, kt=kt, vi=vi: e.matmul(
                                            ps[:, accb + u // 2, (u % 2) * VW:(u % 2) * VW + VW],
                                            lhsT=pT[:, ls, kl * 4 + u, :], rhs=vres[:, kt, vi, :],
                                            start=(kt == 0 and u % 2 == 0), stop=(kt == NK - 1), skip_group_check=True),
                                            [('pT', ls), ('vres', VLAST), 'vres'], [('acc', s)])

                            rec_logits(0)
                            for kp in range(1, NP):
                                rec_logits(kp)
                                rec_av(kp - 1)
                            rec_av(NP - 1)
                            def accv(u, lo, hi):
                                return ps[:, accb + u // 2, (u % 2) * VW + lo:(u % 2) * VW + hi]
                            if kind == 'dsa':
                                for u in range(4):
                                    P.add('dve', lambda e, s=s, u=u: e.reciprocal(out=fs[:, s, u:u + 1], in_=accv(u, 64, 65)),
                                          [('acc', s)], [('fs', s, u)])
                                    P.add('dve', lambda e, s=s, u=u: e.tensor_scalar(out=mix[:, s, u * 64:(u + 1) * 64], in0=accv(u, 0, 64),
                                                                                    scalar1=fs[:, s, u:u + 1], scalar2=None, op0=ALU.mult),
                                          [('acc', s), ('fs', s, u)], [('mix', s)])
                            else:
                                for hl in range(2):
                                    u0, u1 = 2 * hl, 2 * hl + 1
                                    f = lambda k: fs[:, s, hl * 8 + k:hl * 8 + k + 1]
                                    P.add('dve', lambda e, f=f, u0=u0: e.reciprocal(out=f(0), in_=accv(u0, 128, 129)),
                                          [('acc', s)], [('fs', s, hl, 0)])
                                    P.add('dve', lambda e, f=f, u1=u1: e.reciprocal(out=f(1), in_=accv(u1, 128, 129)),
                                          [('acc', s)], [('fs', s, hl, 1)])
                                    P.add('dve', lambda e, f=f: e.tensor_tensor(out=f(2), in0=f(1), in1=neglam, op=ALU.mult),
                                          [('fs', s, hl, 1), 'neglam'], [('fs', s, hl, 2)])
                                    P.add('dve', lambda e, f=f, u1=u1, s=s: e.tensor_scalar(out=ftmp[:, s, :], in0=accv(u1, 0, 128), scalar1=f(2),
                                                                                           scalar2=None, op0=ALU.mult),
                                          [('acc', s), ('fs', s, hl, 2)], [('ftmp', s)])
                                    P.add('dve', lambda e, f=f, u0=u0, s=s: e.scalar_tensor_tensor(out=ftmp[:, s, :], in0=accv(u0, 0, 128), scalar=f(0),
                                                                                                  in1=ftmp[:, s, :], op0=ALU.mult, op1=ALU.add),
                                          [('acc', s), ('fs', s, hl, 0), ('ftmp', s)], [('ftmp2', s)])
                                    P.add('dve', lambda e, f=f, s=s: e.scalar_tensor_tensor(out=junk[:, 0:128], in0=ftmp[:, s, :], scalar=1.0,
                                                                                           in1=ftmp[:, s, :], op0=ALU.mult, op1=ALU.mult,
                                                                                           accum_out=f(3)),
                                          [('ftmp2', s)], ['junk', ('fs', s, hl, 3)])
                                    P.add('dve', lambda e, f=f: e.tensor_scalar(out=f(4), in0=f(3), scalar1=1.0 / 128, scalar2=EPS,
                                                                                op0=ALU.mult, op1=ALU.add),
                                          [('fs', s, hl, 3)], [('fs', s, hl, 4)])
                                    P.add('pool', lambda e, f=f: e.tensor_tensor(out=f(5), in0=f(4), in1=mhalf[:, 0:1], op=ALU.pow),
                                          [('fs', s, hl, 4), 'mhalf'], [('fs', s, hl, 5)])
                                    P.add('dve', lambda e, f=f, s=s, hl=hl: e.scalar_tensor_tensor(
                                        out=mix[:, s, hl * 128:(hl + 1) * 128], in0=ftmp[:, s, :], scalar=f(5), in1=gsub[:],
                                        op0=ALU.mult, op1=ALU.mult), [('ftmp2', s), ('fs', s, hl, 5), 'gsub'], [('mix', s)])
                            mc0 = (0 if kind == 'dsa' else 512) + grp * 256
                            P.add('sp', lambda e, s=s, t0=t0, mc0=mc0: e.dma_start(out=mixed[t0:t0 + 128, mc0:mc0 + 256], in_=mix[:, s, :]),
                                  [('mix', s)], ['d_mixed'], dma='smix%d' % s)
                    P.barrier()

                for kind in ('dsa', 'diff'):
                    for grp in range(2):
                        attn_pass(kind, grp)

                with contextlib.ExitStack() as st6:
                    def sb6(name, shape, dt):
                        return st6.enter_context(nc.sbuf_tensor(un(name), list(shape), dt))
                    wo = sb6("wo", [128, 8, D], BF)
                    wst = sb6("wst6", [128, 2, 842], F32)
                    xt = sb6("xt6", [128, 2, D], F32)
                    mt = sb6("mt6", [128, 2, D], BF)
                    mT = sb6("mT6", [128, 2, 8, 128], BF)
                    for kc in range(8):
                        load_weight(wo[:, kc, :], w_out[L, kc * 128:(kc + 1) * 128, :], D, wst, 'wo', chunk=512)
                    for it in range(NT):
                        s = it % 2
                        t0 = it * 128
                        P.add('sp', lambda e, s=s, t0=t0: e.dma_start(out=xt[:, s, :], in_=xsrc[t0:t0 + 128, :]),
                              (), [('xt', s)], dma='xt%d' % s)
                        P.add('sp', lambda e, s=s, t0=t0: e.dma_start(out=mt[:, s, :], in_=mixed[t0:t0 + 128, :]),
                              (), [('mt', s)], dma='mt%d' % s)
                        for kc in range(8):
                            P.add('pe', lambda e, s=s, kc=kc: e.transpose(out=psb(0)[:, kc * 128:(kc + 1) * 128],
                                                                          in_=mt[:, s, kc * 128:(kc + 1) * 128], identity=ident[:]),
                                  [('mt', s), 'ident'], ['pb0'])
                        P.add('act', lambda e, s=s: e.copy(out=mT[:, s].rearrange("p a b -> p (a b)"), in_=psb(0)[:, :]),
                              ['pb0'], [('mT', s)])
                        pb = 2 + 2 * s
                        for cg in range(2):
                            for kc in range(8):
                                P.add('pe', lambda e, s=s, kc=kc, cg=cg, pb=pb: e.matmul(
                                    ps[:, pb + cg, :], lhsT=mT[:, s, kc, :], rhs=wo[:, kc, cg * 512:(cg + 1) * 512],
                                    start=(kc == 0), stop=(kc == 7)), [('mT', s), 'wo'], [('po', s)])
                        P.add('dve', lambda e, s=s, pb=pb: e.tensor_tensor(out=xt[:, s, :], in0=ps[:, pb:pb + 2, :].rearrange("p a b -> p (a b)"),
                                                                           in1=xt[:, s, :], op=ALU.add),
                              [('po', s), ('xt', s)], [('x1', s)])
                        P.add('dve', lambda e, s=s, it=it: e.scalar_tensor_tensor(out=junk[:, :], in0=xt[:, s, :], scalar=1.0, in1=xt[:, s, :],
                                                                                  op0=ALU.mult, op1=ALU.mult, accum_out=ssA[:, it:it + 1]),
                              [('x1', s), ('xt', s)], ['junk', 'ss'])
                        P.add('sp', lambda e, s=s, t0=t0: e.dma_start(out=xres[t0:t0 + 128, :], in_=xt[:, s, :]),
                              [('x1', s)], ['d_xres', ('xt', s)], dma='sx%d' % s)
                    rstd_from_ss(ssA[:, :], rstd[:, :], NT, 1.0 / D)
                    P.barrier()

                with contextlib.ExitStack() as st7:
                    def sb7(name, shape, dt):
                        return st7.enter_context(nc.sbuf_tensor(un(name), list(shape), dt))
                    wg = sb7("wg", [128, 8, DFF], BF)
                    wu = sb7("wu", [128, 8, DFF], BF)
                    wd = sb7("wd", [128, 22, D], BF)
                    wst = sb7("wst7", [128, 2, 512], F32)
                    gbc = sb7("gbc7", [128, D], F32)
                    gfc = sb7("gfc7", [128, D], F32)
                    xt = sb7("xt7", [128, 2, D], F32)
                    hb = sb7("hb7", [128, D], BF)
                    hT = sb7("hT7", [128, 8, 128], BF)
                    sg = sb7("sg7", [128, 2, 512], F32)
                    gb = sb7("gb7", [128, DFF], BF)
                    gT = sb7("gT7", [128, 22, 128], BF)
                    rs2 = sb7("rs27", [128, 2, 4], F32)
                    P.add('sp', lambda e, L=L: e.dma_start(out=gbc[:], in_=g_ffn[L:L + 1, :].partition_broadcast(128)),
                          (), ['gbc'], dma='c2')
                    P.add('sp', lambda e: e.dma_start(out=gfc[:], in_=g_final[0:1, :].partition_broadcast(128)),
                          (), ['gfc'], dma='c3')
                    for kc in range(8):
                        load_weight(wg[:, kc, :], w_gate[L, kc * 128:(kc + 1) * 128, :], DFF, wst, 'wg', chunk=512)
                        load_weight(wu[:, kc, :], w_up[L, kc * 128:(kc + 1) * 128, :], DFF, wst, 'wu', chunk=512)
                    for kc in range(22):
                        load_weight(wd[:, kc, :], w_down[L, kc * 128:(kc + 1) * 128, :], D, wst, 'wd', chunk=512)
                    for it in range(NT):
                        s = it % 2
                        t0 = it * 128
                        P.add('sp', lambda e, s=s, t0=t0: e.dma_start(out=xt[:, s, :], in_=xres[t0:t0 + 128, :]),
                              ['d_xres'], [('xt', s)], dma='xt%d' % s)
                        P.add('dve', lambda e, s=s, it=it: e.scalar_tensor_tensor(out=hb[:, :], in0=xt[:, s, :], scalar=rstd[:, it:it + 1],
                                                                                  in1=gbc[:], op0=ALU.mult, op1=ALU.mult),
                              [('xt', s), 'rstd', 'gbc'], ['hb'])
                        for kc in range(8):
                            P.add('pe', lambda e, kc=kc: e.transpose(out=psb(0)[:, kc * 128:(kc + 1) * 128],
                                                                     in_=hb[:, kc * 128:(kc + 1) * 128], identity=ident[:]),
                                  ['hb', 'ident'], [('pb', 0)])
                        P.add('act', lambda e: e.copy(out=hT[:].rearrange("p a b -> p (a b)"), in_=psb(0)[:, :]),
                              [('pb', 0)], ['hT'])
                        for cg in range(6):
                            c0 = cg * 512
                            n = min(512, DFF - c0)
                            q = cg % 2
                            bg, bu = 2 + 2 * q, 3 + 2 * q
                            for kc in range(8):
                                P.add('pe', lambda e, kc=kc, c0=c0, n=n, bg=bg: e.matmul(ps[:, bg, 0:n], lhsT=hT[:, kc, :], rhs=wg[:, kc, c0:c0 + n],
                                                                                         start=(kc == 0), stop=(kc == 7)), ['hT', 'wg'], [('pb', bg)])
                            for kc in range(8):
                                P.add('pe', lambda e, kc=kc, c0=c0, n=n, bu=bu: e.matmul(ps[:, bu, 0:n], lhsT=hT[:, kc, :], rhs=wu[:, kc, c0:c0 + n],
                                                                                         start=(kc == 0), stop=(kc == 7)), ['hT', 'wu'], [('pb', bu)])
                            P.add('act', lambda e, q=q, n=n, bg=bg: e.activation(out=sg[:, q, 0:n], in_=ps[:, bg, 0:n], func=AF.Silu),
                                  [('pb', bg)], [('sg', q)])
                            P.add('dve', lambda e, q=q, n=n, bu=bu, c0=c0: e.tensor_tensor(out=gb[:, c0:c0 + n], in0=ps[:, bu, 0:n], in1=sg[:, q, 0:n],
                                                                                          op=ALU.mult), [('pb', bu), ('sg', q)], ['gb'])
                        for rnd in range(3):
                            lo, hi = rnd * 8, min(rnd * 8 + 8, 22)
                            tb = rnd % 2
                            for k in range(lo, hi):
                                P.add('pe', lambda e, k=k, lo=lo, tb=tb: e.transpose(out=psb(tb)[:, (k - lo) * 128:(k - lo + 1) * 128],
                                                                                     in_=gb[:, k * 128:(k + 1) * 128], identity=ident[:]),
                                      ['gb', 'ident'], [('pb', tb)])
                            P.add('act', lambda e, lo=lo, hi=hi, tb=tb: e.copy(out=gT[:, lo:hi, :].rearrange("p a b -> p (a b)"),
                                                                               in_=psb(tb)[:, 0:(hi - lo) * 128]), [('pb', tb)], ['gT'])
                        for cg in range(2):
                            for kc in range(22):
                                P.add('pe', lambda e, kc=kc, cg=cg: e.matmul(ps[:, 6 + cg, :], lhsT=gT[:, kc, :], rhs=wd[:, kc, cg * 512:(cg + 1) * 512],
                                                                             start=(kc == 0), stop=(kc == 21)), ['gT', 'wd'], ['pdn'])
                        P.add('dve', lambda e, s=s: e.tensor_tensor(out=xt[:, s, :], in0=ps[:, 6:8, :].rearrange("p a b -> p (a b)"),
                                                                    in1=xt[:, s, :], op=ALU.add), ['pdn', ('xt', s)], [('x2', s)])
                        if L < DEPTH - 1:
                            P.add('dve', lambda e, s=s, it=it: e.scalar_tensor_tensor(out=junk[:, :], in0=xt[:, s, :], scalar=1.0, in1=xt[:, s, :],
                                                                                      op0=ALU.mult, op1=ALU.mult, accum_out=ssA[:, it:it + 1]),
                                  [('x2', s), ('xt', s)], ['junk', 'ss2'])
                            P.add('sp', lambda e, s=s, t0=t0: e.dma_start(out=xres[t0:t0 + 128, :], in_=xt[:, s, :]),
                                  [('x2', s)], ['d_xres2', ('xt', s)], dma='sx%d' % s)
                        else:
                            P.add('dve', lambda e, s=s: e.scalar_tensor_tensor(out=junk[:, :], in0=xt[:, s, :], scalar=1.0, in1=xt[:, s, :],
                                                                               op0=ALU.mult, op1=ALU.mult, accum_out=rs2[:, s, 0:1]),
                                  [('x2', s), ('xt', s)], ['junk', ('rs2', s, 0)])
                            P.add('dve', lambda e, s=s: e.tensor_scalar(out=rs2[:, s, 1:2], in0=rs2[:, s, 0:1], scalar1=1.0 / D, scalar2=EPS,
                                                                        op0=ALU.mult, op1=ALU.add), [('rs2', s, 0)], [('rs2', s, 1)])
                            P.add('pool', lambda e, s=s: e.tensor_tensor(out=rs2[:, s, 2:3], in0=rs2[:, s, 1:2], in1=mhalf[:, 0:1], op=ALU.pow),
                                  [('rs2', s, 1), 'mhalf'], [('rs2', s, 2)])
                            P.add('dve', lambda e, s=s: e.scalar_tensor_tensor(out=xt[:, s, :], in0=xt[:, s, :], scalar=rs2[:, s, 2:3], in1=gfc[:],
                                                                               op0=ALU.mult, op1=ALU.mult),
                                  [('x2', s), ('rs2', s, 2), 'gfc'], [('yo', s)])
                            P.add('sp', lambda e, s=s, t0=t0: e.dma_start(out=y_out[t0:t0 + 128, :], in_=xt[:, s, :]),
                                  [('yo', s)], ['d_y', ('xt', s)], dma='sx%d' % s)
                    if L < DEPTH - 1:
                        rstd_from_ss(ssA[:, :], rstd[:, :], NT, 1.0 / D)
                    P.barrier()

        except _Stop:
            pass
        P.emit(nc, stack)
    return nc


def _consts(S, r):
    TPC = S // 2
    NT = TPC // 128
    pos = np.concatenate([np.arange(128) + (2 * i + r) * 128 for i in range(NT)]).astype(np.float32)
    def tab(dim):
        inv = (10000.0 ** (-np.arange(0, dim, 2, dtype=np.float32) / dim)).astype(np.float32)
        ang = pos[:, None] * inv[None, :]
        return np.cos(ang).astype(np.float32), np.sin(ang).astype(np.float32)
    c64, s64 = tab(64)
    c32, s32 = tab(32)
    rope = np.concatenate([c64, s64, c32, s32], axis=1).astype(np.float32)
    t = np.arange(128)[:, None] // 64
    s_ = np.arange(128)[None, :] // 64
    diag = (s_ <= t)
    full = np.ones((128, 128), bool)
    none = np.zeros((128, 128), bool)
    allow = np.concatenate([diag, none], axis=1) if r == 0 else np.concatenate([full, diag], axis=1)
    cbf = np.where(allow, 0.0, -1e30).astype(np.float32)
    cbb = np.where(allow, 0.0, NEG).astype(np.float32).astype(ml_dtypes.bfloat16)
    ident = np.eye(128, dtype=np.float32).astype(ml_dtypes.bfloat16)
    pow2 = np.tile((2.0 ** -np.arange(32, dtype=np.float32))[None, :], (128, 1)).astype(np.float32)
    return dict(rope=rope, cbf=cbf, cbb=cbb, ident=ident, pow2=pow2)


_NC_CACHE = {}


def _prep(x, w_in, w_out, g_mix, lam_q1, lam_k1, lam_q2, lam_k2, g_subln, g_ffn, w_gate, w_up, w_down, g_final):
    x = np.asarray(x, dtype=np.float32)
    B, S, _ = x.shape
    assert B == 4
    TPC = S // 2
    f = lambda a: np.ascontiguousarray(np.asarray(a, dtype=np.float32))
    lamv = np.ascontiguousarray(np.stack([f(lam_q1), f(lam_k1), f(lam_q2), f(lam_k2)], axis=1))
    shared = dict(w_in=f(w_in), w_out=f(w_out), w_gate=f(w_gate), w_up=f(w_up), w_down=f(w_down), g_mix=f(g_mix),
                  g_ffn=f(g_ffn), g_final=f(g_final).reshape(1, D), g_subln=f(g_subln), lamv=lamv)
    in_maps = []
    for c in range(8):
        b, r = c // 2, c % 2
        xb = x[b].reshape(S // 128, 128, D)
        xl = np.ascontiguousarray(xb[r::2].reshape(TPC, D))
        m = dict(shared)
        m["x"] = xl
        m.update(_consts(S, r))
        in_maps.append(m)
    return in_maps, B, S


def _gather(ys, B, S):
    NT = S // 256
    out = np.empty((B, S, D), dtype=np.float32)
    for c in range(8):
        b, r = c // 2, c % 2
        yl = np.asarray(ys[c], dtype=np.float32).reshape(NT, 128, D)
        out[b].reshape(S // 128, 128, D)[r::2] = yl
    return out


def kernel(x, w_in, w_out, g_mix, lam_q1, lam_k1, lam_q2, lam_k2, g_subln, g_ffn, w_gate, w_up, w_down, g_final):
    in_maps, B, S = _prep(x, w_in, w_out, g_mix, lam_q1, lam_k1, lam_q2, lam_k2, g_subln, g_ffn, w_gate, w_up, w_down, g_final)
    if S not in _NC_CACHE:
        _NC_CACHE[S] = build(S)
    nc = _NC_CACHE[S]
    res = run_bass_kernel_spmd(nc, in_maps, core_ids=list(range(8)))
    return _gather([res.results[c]["y"] for c in range(8)], B, S)
```

```python
import bisect
import contextlib
import math
import sys
import numpy as np
import ml_dtypes
import concourse.bass as bass
import concourse.mybir as mybir
from concourse.bass_utils import run_bass_kernel_spmd

F32 = mybir.dt.float32
BF = mybir.dt.bfloat16
AF = mybir.ActivationFunctionType
ALU = mybir.AluOpType

D = 1024
DIN = 3368
DFF = 2816
DEPTH = 2
EPS = 1e-6
NBIS = 12
NEG = -30000.0
ENGS = ['pe', 'act', 'dve', 'pool', 'sp']


class Op:
    pass


class _Stop(Exception):
    pass


STOP = None
INSMAP = None
FULLSYNC = True
P3MODE = 3
NAMES = {}


class Rec:
    def __getattr__(self, name):
        def f(*a, **k):
            self.call = (name, a, k)
            return self
        return f


class Prog:
    def __init__(self):
        self.ops = []
        self.lastw = {}
        self.rd = {}
        self.bar = []
        self.since_dma = []
        self.last_eng = {}

    def add(self, eng, fn, reads=(), writes=(), dma=None, cc=False):
        if getattr(self, 'stopped', False):
            return None
        o = Op()
        rec = Rec(); fn(rec)
        o.eng = eng; o.fn = rec.call; o.dma = dma; o.cc = cc
        o.deps = {}; o.needed = False; o.id = len(self.ops); o.sig = None
        o.line = sys._getframe(1).f_lineno

        def dep(d, kind):
            if d is None or d is o:
                return
            if d.dma is None and o.dma is None and d.eng == eng:
                if eng == 'pe' or (kind == 'war' and not FULLSYNC):
                    return
            o.deps[d.id] = d

        for r in reads:
            dep(self.lastw.get(r), 'raw')
        for r in writes:
            dep(self.lastw.get(r), 'waw')
            for q in self.rd.get(r, ()):
                dep(q, 'war')
        for b in self.bar:
            dep(b, 'war')
        for r in reads:
            self.rd.setdefault(r, []).append(o)
        for r in writes:
            self.lastw[r] = o
            self.rd[r] = []
        for d in o.deps.values():
            d.needed = True
        self.ops.append(o)
        if dma is not None:
            self.since_dma.append(o)
        else:
            self.last_eng[eng] = o
        return o

    def barrier(self):
        self.bar = list(self.last_eng.values()) + list(self.since_dma)
        self.since_dma = []
        self.nbar = getattr(self, 'nbar', 0) + 1
        if STOP is not None and self.nbar > STOP:
            self.stopped = True

    def emit(self, nc, stack):
        chans = sorted({o.dma for o in self.ops if o.dma is not None and not o.cc})
        esem = {e: stack.enter_context(nc.semaphore("e_" + e)) for e in ENGS}
        csem = {c: stack.enter_context(nc.semaphore("c_" + c)) for c in chans}
        cnt = {e: 0 for e in ENGS}
        chn = {c: 0 for c in chans}
        for o in self.ops:
            if o.cc:
                o.sig = (stack.enter_context(nc.semaphore("cc%d" % o.id)), 1)
            elif o.dma is not None:
                chn[o.dma] += 1
                o.sig = (csem[o.dma], 16 * chn[o.dma])
            elif o.needed:
                cnt[o.eng] += 1
                o.sig = (esem[o.eng], cnt[o.eng])
        per = {e: [o for o in self.ops if o.eng == e] for e in ENGS}
        chan_ids = {c: [o.id for o in self.ops if o.dma == c and not o.cc] for c in chans}
        block = stack.enter_context(nc.Block())

        def run(e, eng):
            waited = {}
            for o in per[e]:
                need = {}
                for d in o.deps.values():
                    sem, val = d.sig
                    if d.dma is not None and not d.cc:
                        val = 16 * bisect.bisect_left(chan_ids[d.dma], o.id)
                    k = id(sem)
                    if need.get(k, (None, 0))[1] < val:
                        need[k] = (sem, val)
                for k, (sem, val) in need.items():
                    if waited.get(k, 0) < val:
                        eng.wait_ge(sem, val)
                        waited[k] = val
                nm, a_, k_ = o.fn
                ins = getattr(eng, nm)(*a_, **k_)
                if INSMAP is not None:
                    try:
                        INSMAP[str(ins.ins.name)] = o.line
                    except Exception:
                        pass
                if o.cc:
                    ins.then_inc(o.sig[0])
                elif o.dma is not None:
                    ins.then_inc(o.sig[0], 16)
                elif o.needed:
                    ins.then_inc(o.sig[0], 1)
            if e == 'sp':
                mx = {}
                for o in self.ops:
                    if o.sig is not None:
                        sem, val = o.sig
                        if mx.get(id(sem), (None, 0))[1] < val:
                            mx[id(sem)] = (sem, val)
                for k, (sem, val) in mx.items():
                    if waited.get(k, 0) < val:
                        eng.wait_ge(sem, val)

        block.tensor(lambda eng: run('pe', eng))
        block.scalar(lambda eng: run('act', eng))
        block.vector(lambda eng: run('dve', eng))
        block.gpsimd(lambda eng: run('pool', eng))
        block.sync(lambda eng: run('sp', eng))


def build(S):
    TPC = S // 2
    NT = TPC // 128
    A8 = TPC // 512
    nc = bass.Bass("TRN2", target_bir_lowering=False)
    P = Prog()

    def din(name, shape, dt=F32):
        return nc.dram_tensor(name, list(shape), dt, kind="ExternalInput").ap()

    x_in = din("x", [TPC, D])
    w_in = din("w_in", [DEPTH, D, DIN])
    w_out = din("w_out", [DEPTH, D, D])
    w_gate = din("w_gate", [DEPTH, D, DFF])
    w_up = din("w_up", [DEPTH, D, DFF])
    w_down = din("w_down", [DEPTH, DFF, D])
    g_mix = din("g_mix", [DEPTH, D])
    g_ffn = din("g_ffn", [DEPTH, D])
    g_final = din("g_final", [1, D])
    g_subln = din("g_subln", [DEPTH, 128])
    lamv = din("lamv", [DEPTH, 4, 64])
    rope = din("rope", [TPC, 96])
    ident_in = din("ident", [128, 128], BF)
    cbf_in = din("cbf", [128, 256])
    cbb_in = din("cbb", [128, 256], BF)
    pow2_in = din("pow2", [128, 32])
    y_out = nc.dram_tensor("y", [TPC, D], F32, kind="ExternalOutput").ap()

    def dscr(name, shape, dt=BF):
        return nc.dram_tensor(name, list(shape), dt)

    xres = dscr("xres", [TPC, D], F32).ap()
    mixed = dscr("mixed", [TPC, D], BF).ap()
    qaT = dscr("qaT", [512, TPC]).ap().rearrange("(c p) t -> p c t", p=128)
    qbT = dscr("qbT", [512, TPC]).ap().rearrange("(c p) t -> p c t", p=128)
    iqT = dscr("iqT", [384, TPC]).ap().rearrange("(c p) t -> p c t", p=128)
    mbs = dscr("mbs", [NT, 128, S]).ap()
    UR = 256 * TPC // 512
    kT_loc = [dscr("kTl%d" % u, [UR, 512]) for u in range(4)]
    kT_all = [dscr("kTa%d" % u, [2 * UR, 512]) for u in range(4)]
    v_loc = [dscr("vl%d" % u, [TPC // 2, 512]) for u in range(4)]
    v_all = [dscr("va%d" % u, [TPC, 512]) for u in range(4)]
    IR = max(32 * TPC // 512, 32)
    ik_loc = dscr("ikl", [IR, 512])
    ik_all = dscr("ika", [2 * IR, 512])

    def kT_loc_view(u):
        return kT_loc[u].ap().rearrange("(c p a) b -> p c (a b)", p=128, a=A8)

    def kT_all_view(u):
        return kT_all[u].ap().rearrange("(r c p a) b -> r p c (a b)", r=2, p=128, a=A8)

    def v_all_view(u):
        return v_all[u].ap().rearrange("(r t) b -> r t b", r=2)

    ik_loc_v = ik_loc.ap().rearrange("(p a) b -> p (a b)", a=A8)
    ik_all_v = ik_all.ap().rearrange("(r p a) b -> r p (a b)", r=2, a=A8)

    stack = contextlib.ExitStack()
    with stack:
        uid = [0]

        def un(name):
            uid[0] += 1
            NAMES.setdefault(name, []).append("s%d_%s" % (uid[0], name))
            return "s%d_%s" % (uid[0], name)

        def sb(name, shape, dt):
            return stack.enter_context(nc.sbuf_tensor(un(name), list(shape), dt))

        ps = stack.enter_context(nc.psum_tensor("ps", [128, 8, 512], F32))

        def psb(b0, nb=1):
            return ps[:, b0, :].bitcast(BF)

        ident = sb("ident", [128, 128], BF)
        ident4 = sb("ident4", [128, 512], BF)
        cbf = sb("cbf", [128, 256], F32)
        cbb = sb("cbb", [128, 256], BF)
        pow2 = sb("pow2", [128, 32], F32)
        mhalf = sb("mhalf", [128, 64], F32)
        ssA = sb("ssA", [128, NT], F32)
        rstd = sb("rstd", [128, NT], F32)
        iw_all = sb("iw_all", [128, NT, 8], F32)
        lam_t = sb("lam_t", [128, 4, 64], F32)
        lam_s = sb("lam_s", [128, 8], F32)
        gsub = sb("gsub", [128, 128], F32)
        junk = sb("junk", [128, 1024], BF)
        P.add('sp', lambda e: e.dma_start(out=ident[:], in_=ident_in), (), ['ident'], dma='c0')
        for q_ in range(4):
            P.add('pool', lambda e, q_=q_: e.tensor_copy(out=ident4[:, q_ * 128:(q_ + 1) * 128], in_=ident[:]), ['ident'], ['ident4'])
        P.add('sp', lambda e: e.dma_start(out=cbf[:], in_=cbf_in), (), ['cbf'], dma='c1')
        P.add('sp', lambda e: e.dma_start(out=cbb[:], in_=cbb_in), (), ['cbb'], dma='c2')
        P.add('sp', lambda e: e.dma_start(out=pow2[:], in_=pow2_in), (), ['pow2'], dma='c3')
        P.add('pool', lambda e: e.memset(mhalf[:], -0.5), (), ['mhalf'])

        def load_weight(dst, src_rows, ncols, wst, tag, chunk=842):
            c0 = 0
            k = load_weight.k
            while c0 < ncols:
                n = min(chunk, ncols - c0)
                s = k % 2
                P.add('sp', lambda e, s=s, c0=c0, n=n: e.dma_start(out=wst[:, s, 0:n], in_=src_rows[:, c0:c0 + n]),
                      (), [('wst', s)], dma='wst%d' % s)
                P.add('pool', lambda e, s=s, c0=c0, n=n: e.tensor_copy(out=dst[:, c0:c0 + n], in_=wst[:, s, 0:n]),
                      [('wst', s)], [tag])
                c0 += n
                k += 1
            load_weight.k = k
        load_weight.k = 0

        def rstd_from_ss(ss_ap, rstd_ap, n, inv):
            P.add('dve', lambda e: e.tensor_scalar(out=ss_ap, in0=ss_ap, scalar1=inv, scalar2=EPS,
                                                   op0=ALU.mult, op1=ALU.add), ['ss'], ['ss'])
            P.add('pool', lambda e: e.tensor_tensor(out=rstd_ap, in0=ss_ap, in1=mhalf[:, 0:n], op=ALU.pow),
                  ['ss', 'mhalf'], ['rstd'])

        with contextlib.ExitStack() as st0:
            xt = st0.enter_context(nc.sbuf_tensor(un("xt_s"), [128, 2, D], F32))
            for it in range(NT):
                s = it % 2
                P.add('sp', lambda e, s=s, it=it: e.dma_start(out=xt[:, s, :], in_=x_in[it * 128:(it + 1) * 128, :]),
                      (), [('xt', s)], dma='xt%d' % s)
                P.add('act', lambda e, s=s, it=it: e.activation(out=junk[:, :], in_=xt[:, s, :], func=AF.Square,
                                                                accum_out=ssA[:, it:it + 1]),
                      [('xt', s)], ['ss', 'junk'])
            rstd_from_ss(ssA[:, :], rstd[:, :], NT, 1.0 / D)
            P.barrier()

        try:
            for L in range(DEPTH if STOP is None or STOP > 0 else 0):
                xsrc = x_in if L == 0 else xres
                lam_init = 0.8 - 0.6 * math.exp(-0.3 * L)
                P.add('sp', lambda e, L=L: e.dma_start(out=lam_t[:].rearrange("p a b -> p (a b)"),
                                                       in_=lamv[L:L + 1].rearrange("o a b -> o (a b)").partition_broadcast(128)),
                      (), ['lam_t'], dma='c0')
                P.add('sp', lambda e, L=L: e.dma_start(out=gsub[:], in_=g_subln[L:L + 1, :].partition_broadcast(128)),
                      (), ['gsub'], dma='c1')
                for j in range(2):
                    P.add('dve', lambda e, j=j: e.scalar_tensor_tensor(out=junk[:, 0:64], in0=lam_t[:, 2 * j, :], scalar=1.0,
                                                                      in1=lam_t[:, 2 * j + 1, :], op0=ALU.mult, op1=ALU.mult,
                                                                      accum_out=lam_s[:, j:j + 1]),
                          ['lam_t'], ['junk', 'lam_s'])
                P.add('act', lambda e: e.activation(out=lam_s[:, 2:4], in_=lam_s[:, 0:2], func=AF.Exp), ['lam_s'], ['lam_e'])
                P.add('dve', lambda e: e.tensor_tensor(out=lam_s[:, 4:5], in0=lam_s[:, 3:4], in1=lam_s[:, 2:3], op=ALU.subtract),
                      ['lam_e'], ['lam_d'])
                P.add('dve', lambda e, li=lam_init: e.tensor_scalar(out=lam_s[:, 5:6], in0=lam_s[:, 4:5], scalar1=-li, scalar2=None,
                                                                    op0=ALU.add), ['lam_d'], ['neglam'])
                P.add('dve', lambda e, li=lam_init: e.tensor_scalar(out=gsub[:], in0=gsub[:], scalar1=1.0 - li, scalar2=None,
                                                                    op0=ALU.mult), ['gsub'], ['gsub'])
                neglam = lam_s[:, 5:6]

                with contextlib.ExitStack() as st1:
                    def sb1(name, shape, dt):
                        return st1.enter_context(nc.sbuf_tensor(un(name), list(shape), dt))
                    wbf = sb1("wbf", [128, 8, DIN], BF)
                    wst = sb1("wst1", [128, 2, 842], F32)
                    gbc = sb1("gbc", [128, D], F32)
                    xt = sb1("xt", [128, 2, D], F32)
                    rt = sb1("rt", [128, 2, 96], F32)
                    hb = sb1("hb", [128, 2, D], BF)
                    hT = sb1("hT", [128, 2, 8, 128], BF)
                    pr = sb1("pr", [128, 2, DIN], F32)
                    prb = sb1("prb", [128, 2, DIN], BF)
                    tqs = sb1("tqs", [128, 2, 24, 128], BF)
                    tA = sb1("tA", [128, 2, 16, 32], F32)
                    tB = sb1("tB", [128, 2, 16, 32], F32)
                    for s_ in range(2):
                        P.add('pool', lambda e, s_=s_: e.memset(tqs[:, s_, 8:12, :].rearrange("p a b -> p (a b)"), 0.0), (),
                              [('tqs', s_, 1, 8), ('tqs', s_, 1, 10), ('tqs', s_, 1, 11)])
                    P.add('sp', lambda e, L=L: e.dma_start(out=gbc[:], in_=g_mix[L:L + 1, :].partition_broadcast(128)),
                          (), ['gbc'], dma='c2')
                    for kc in range(8):
                        load_weight(wbf[:, kc, :], w_in[L, kc * 128:(kc + 1) * 128, :], DIN, wst, 'wbf')
                    tch = [(c * 128, 128) for c in range(8)]
                    tch += [(1536, 96), (1632, 96), (1728, 64)]
                    tch += [(1792, 32)]
                    tch += [(1832 + c * 128, 128) for c in range(8)]
                    for it in range(NT):
                        s = it % 2
                        t0 = it * 128
                        P.add('sp', lambda e, s=s, t0=t0: e.dma_start(out=xt[:, s, :], in_=xsrc[t0:t0 + 128, :]),
                              (), [('xt', s)], dma='xt%d' % s)
                        P.add('sp', lambda e, s=s, t0=t0: e.dma_start(out=rt[:, s, :], in_=rope[t0:t0 + 128, :]),
                              (), [('rt', s)], dma='rt%d' % s)
                        P.add('dve', lambda e, s=s, it=it: e.scalar_tensor_tensor(out=hb[:, s, :], in0=xt[:, s, :],
                                                                                   scalar=rstd[:, it:it + 1], in1=gbc[:],
                                                                                   op0=ALU.mult, op1=ALU.mult),
                              [('xt', s), 'rstd', 'gbc'], [('hb', s)])
                        for kc in range(8):
                            P.add('pe', lambda e, s=s, kc=kc: e.transpose(out=psb(0)[:, kc * 128:(kc + 1) * 128],
                                                                          in_=hb[:, s, kc * 128:(kc + 1) * 128], identity=ident[:]),
                                  [('hb', s), 'ident'], ['pb0'])
                        P.add('act', lambda e, s=s: e.copy(out=hT[:, s].rearrange("p a b -> p (a b)"), in_=psb(0)[:, :]),
                              ['pb0'], [('hT', s)])
                        ncg = (DIN + 511) // 512
                        for cg in range(ncg):
                            c0 = cg * 512
                            n = min(512, DIN - c0)
                            b = 1 + cg % 2
                            for kc in range(8):
                                P.add('pe', lambda e, s=s, kc=kc, c0=c0, n=n, b=b: e.matmul(
                                    ps[:, b, 0:n], lhsT=hT[:, s, kc, :], rhs=wbf[:, kc, c0:c0 + n],
                                    start=(kc == 0), stop=(kc == 7)), [('hT', s), 'wbf'], [('pb', b)])
                            if cg % 2 == 0:
                                P.add('act', lambda e, s=s, c0=c0, n=n, b=b: e.copy(out=pr[:, s, c0:c0 + n], in_=ps[:, b, 0:n]),
                                      [('pb', b)], [('pr', s, cg)])
                            else:
                                P.add('dve', lambda e, s=s, c0=c0, n=n, b=b: e.tensor_copy(out=pr[:, s, c0:c0 + n], in_=ps[:, b, 0:n]),
                                      [('pb', b)], [('pr', s, cg)])
                        def rope_grp(eng, tmp, c0, H, half, coff, soff, s=s):
                            xv = pr[:, s, c0:c0 + H * 2 * half].rearrange("p (h j d) -> p h j d", h=H, j=2)
                            ov = prb[:, s, c0:c0 + H * 2 * half].rearrange("p (h j d) -> p h j d", h=H, j=2)
                            cosb = rt[:, s, coff:coff + half].unsqueeze(1).to_broadcast([128, H, half])
                            sinb = rt[:, s, soff:soff + half].unsqueeze(1).to_broadcast([128, H, half])
                            t1 = tmp[:, 0, 0:H, 0:half]
                            t2 = tmp[:, 1, 0:H, 0:half]
                            rs_ = [('pr', s, q_) for q_ in range(7)] + [('rt', s)]
                            tg = ('tmp', eng)
                            P.add(eng, lambda e: e.tensor_tensor(out=t1, in0=xv[:, :, 0, :], in1=cosb, op=ALU.mult), rs_, [(tg, 0)])
                            P.add(eng, lambda e: e.tensor_tensor(out=t2, in0=xv[:, :, 1, :], in1=sinb, op=ALU.mult), rs_, [(tg, 1)])
                            P.add(eng, lambda e: e.tensor_tensor(out=ov[:, :, 0, :], in0=t1, in1=t2, op=ALU.subtract),
                                  [(tg, 0), (tg, 1)], [('prb', s, c0)])
                            P.add(eng, lambda e: e.tensor_tensor(out=t1, in0=xv[:, :, 1, :], in1=cosb, op=ALU.mult), rs_, [(tg, 0)])
                            P.add(eng, lambda e: e.tensor_tensor(out=t2, in0=xv[:, :, 0, :], in1=sinb, op=ALU.mult), rs_, [(tg, 1)])
                            P.add(eng, lambda e: e.tensor_tensor(out=ov[:, :, 1, :], in0=t1, in1=t2, op=ALU.add),
                                  [(tg, 0), (tg, 1)], [('prb', s, c0)])
                        rope_grp('dve', tA, 0, 16, 32, 0, 32)
                        rope_grp('pool', tB, 1832, 16, 32, 0, 32)
                        rope_grp('dve', tA, 1536, 9, 16, 64, 80)
                        P.add('pool', lambda e, s=s: e.tensor_copy(out=prb[:, s, 1024:1536], in_=pr[:, s, 1024:1536]),
                              [('pr', s, q_) for q_ in range(7)], [('prb', s, 'v')])
                        P.add('pool', lambda e, s=s: e.tensor_copy(out=prb[:, s, 2856:3368], in_=pr[:, s, 2856:3368]),
                              [('pr', s, q_) for q_ in range(7)], [('prb', s, 'v')])
                        P.add('pool', lambda e, s=s, it=it: e.tensor_copy(out=iw_all[:, it, :], in_=pr[:, s, 1824:1832]),
                              [('pr', s, q_) for q_ in range(7)], ['iw_all'])
                        for bi in range(3):
                            lo, hi = bi * 8, min(bi * 8 + 8, len(tch))
                            for k in range(lo, hi):
                                c0, n = tch[k]
                                P.add('pe', lambda e, s=s, k=k, c0=c0, n=n, bi=bi: e.transpose(
                                    out=psb(3 + bi)[0:n, (k - bi * 8) * 128:(k - bi * 8 + 1) * 128],
                                    in_=prb[:, s, c0:c0 + n], identity=ident[:]), [('prb', s, 0), ('prb', s, 1832), ('prb', s, 1536), 'ident'], [('pb', 3 + bi)])
                            if bi == 1:
                                pieces = [(96, 8, 10), (64, 10, 11), (32, 11, 12), (128, 12, 16)]
                            else:
                                pieces = [(128, lo, hi)]
                            for (nr, a0, a1) in pieces:
                                o0, o1 = (a0 - lo) * 128, (a1 - lo) * 128
                                if bi % 2 == 0:
                                    P.add('act', lambda e, s=s, a0=a0, a1=a1, o0=o0, o1=o1, nr=nr, bi=bi: e.copy(
                                        out=tqs[0:nr, s, a0:a1, :].rearrange("p a b -> p (a b)"), in_=psb(3 + bi)[0:nr, o0:o1]),
                                        [('pb', 3 + bi)], [('tqs', s, bi, a0)])
                                else:
                                    P.add('dve', lambda e, s=s, a0=a0, a1=a1, o0=o0, o1=o1, nr=nr, bi=bi: e.tensor_copy(
                                        out=tqs[0:nr, s, a0:a1, :].rearrange("p a b -> p (a b)"), in_=psb(3 + bi)[0:nr, o0:o1]),
                                        [('pb', 3 + bi)], [('tqs', s, bi, a0)])
                        ch = 'st%d' % s
                        rd_all = [('tqs', s, 0, 0), ('tqs', s, 1, 8), ('tqs', s, 1, 10), ('tqs', s, 1, 11), ('tqs', s, 1, 12), ('tqs', s, 2, 16)]
                        P.add('act', lambda e, s=s, t0=t0: e.dma_start(out=qaT[:, :, t0:t0 + 128], in_=tqs[:, s, 0:4, :]),
                              rd_all, ['d_qaT'], dma=ch)
                        for u in range(2):
                            P.add('act', lambda e, s=s, t0=t0, u=u: e.dma_start(out=kT_loc_view(u)[:, :, t0:t0 + 128],
                                                                                in_=tqs[:, s, 4 + 2 * u:6 + 2 * u, :]),
                                  rd_all, ['d_kT'], dma=ch)
                        P.add('act', lambda e, s=s, t0=t0: e.dma_start(out=iqT[0:96, :, t0:t0 + 128], in_=tqs[0:96, s, 8:11, :]),
                              rd_all, ['d_iqT'], dma=ch)
                        P.add('act', lambda e, s=s, t0=t0: e.dma_start(out=ik_loc_v[:, t0:t0 + 128], in_=tqs[0:32, s, 11, :]),
                              rd_all, ['d_ik'], dma=ch)
                        P.add('act', lambda e, s=s, t0=t0: e.dma_start(out=qbT[:, :, t0:t0 + 128], in_=tqs[:, s, 12:16, :]),
                              rd_all, ['d_qbT'], dma=ch)
                        for u in range(2):
                            P.add('act', lambda e, s=s, t0=t0, u=u: e.dma_start(out=kT_loc_view(2 + u)[:, :, t0:t0 + 128],
                                                                                in_=tqs[:, s, 16 + 2 * u:18 + 2 * u, :]),
                                  rd_all, ['d_kT'], dma=ch)
                        hu = it // (NT // 2)
                        tl = (it % (NT // 2)) * 128
                        P.add('pool', lambda e, s=s, hu=hu, tl=tl: e.dma_start(out=v_loc[hu].ap()[tl:tl + 128, :],
                                                                               in_=prb[:, s, 1024:1536]),
                              [('prb', s, 'v')], ['d_v'], dma='sv%d' % s)
                        P.add('pool', lambda e, s=s, hu=hu, tl=tl: e.dma_start(out=v_loc[2 + hu].ap()[tl:tl + 128, :],
                                                                               in_=prb[:, s, 2856:3368]),
                              [('prb', s, 'v')], ['d_v'], dma='sv%d' % s)
                    P.barrier()

                groups = [[0, 1], [2, 3], [4, 5], [6, 7]]
                for a, b in [(kT_loc[u], kT_all[u]) for u in range(4)] + [(v_loc[u], v_all[u]) for u in range(4)] + [(ik_loc, ik_all)]:
                    P.add('pool', lambda e, a=a, b=b: e.collective_compute("AllGather", ALU.bypass, replica_groups=groups,
                                                                           ins=[a.ap().opt()], outs=[b.ap().opt()]),
                          (), ['gath'], dma='cc', cc=True)
                P.barrier()

                with contextlib.ExitStack() as st3:
                    def sb3(name, shape, dt):
                        return st3.enter_context(nc.sbuf_tensor(un(name), list(shape), dt))
                    ikr = sb3("ikr", [96, 3, S], BF)
                    iqt = sb3("iqt", [96, 2, 3, 128], BF)
                    Rt = sb3("Rt", [128, 2, 8, 256], BF)
                    Dg = sb3("Dg", [128, 2, 8, 128], BF)
                    Ssb = sb3("Ssb", [128, 2, S], F32)
                    cjunk = sb3("cjunk", [128, S], BF)
                    mbt = sb3("mbt", [128, 1, S], BF)
                    sm = sb3("sm", [128, 16], F32)
                    W2 = sb3("W2", [128, 32], F32)
                    P.add('pool', lambda e: e.memset(ikr[:].rearrange("p g k -> p (g k)"), 0.0), (), ['ikr0'])
                    ikv = ikr[:].rearrange("p g (l r k) -> p g l r k", r=2, k=128)
                    for g in range(3):
                        for r in range(2):
                            P.add('sp', lambda e, g=g, r=r: e.dma_start(
                                out=ikv[32 * g:32 * g + 32, g, :, r, :],
                                in_=ik_all_v[r].rearrange("p (l k) -> p l k", k=128)), ['ikr0'], ['ikr'], dma='ikr')
                    for i in range(NT):
                        s = i % 2
                        N = (2 * i + 2) * 128
                        t0 = i * 128
                        P.add('sp', lambda e, s=s, t0=t0: e.dma_start(out=iqt[:, s], in_=iqT[0:96, :, t0:t0 + 128]),
                              (), [('iqt', s)], dma='iqt%d' % s)
                        for h in range(8):
                            P.add('pool', lambda e, s=s, i=i, h=h: e.tensor_scalar(
                                out=Dg[:, s, h, :], in0=ident[:], scalar1=iw_all[:, i, h:h + 1], scalar2=None, op0=ALU.mult),
                                ['ident', 'iw_all'], [('Dg', s)])
                        q = i % 2
                        NG = N // 256

                        def rec_dots(kg, s=s):
                            k0 = kg * 256
                            rk = kg % 2
                            for h in range(8):
                                c, g = h // 3, h % 3
                                P.add('pe', lambda e, h=h, c=c, g=g, k0=k0: e.matmul(
                                    ps[:, h // 2, (h % 2) * 256:(h % 2) * 256 + 256],
                                    lhsT=iqt[0:96, s, c, :], rhs=ikr[0:96, g, k0:k0 + 256],
                                    start=True, stop=True), [('iqt', s), 'ikr'], ['dots'])
                            P.add('act', lambda e, rk=rk: e.activation(out=Rt[:, rk].rearrange("p a b -> p (a b)"),
                                                                       in_=ps[:, 0:4, :].rearrange("p a b -> p (a b)"), func=AF.Relu),
                                  ['dots'], [('Rt', rk)])

                        def rec_score(kg, s=s, q=q):
                            k0 = kg * 256
                            rk = kg % 2
                            sbk = 4 + kg % 2
                            for h in range(8):
                                P.add('pe', lambda e, h=h, sbk=sbk, rk=rk: e.matmul(
                                    ps[:, sbk, 0:256], lhsT=Dg[:, s, h, :], rhs=Rt[:, rk, h, :],
                                    start=(h == 0), stop=(h == 7)), [('Dg', s), ('Rt', rk)], [('pb', sbk)])
                            P.add('act', lambda e, sbk=sbk, k0=k0: e.copy(out=Ssb[:, q, k0:k0 + 256], in_=ps[:, sbk, 0:256]),
                                  [('pb', sbk)], [('Ssb', q)])

                        rec_dots(0)
                        for kg in range(1, NG):
                            rec_dots(kg)
                            rec_score(kg - 1)
                        rec_score(NG - 1)
                        thr = sm[:, 0:1]
                        if i >= 1 and P3MODE >= 2:
                            P.add('dve', lambda e, N=N, q=q: e.tensor_reduce(out=sm[:, 1:2], in_=Ssb[:, q, 0:N], axis=mybir.AxisListType.X,
                                                                        op=ALU.max, apply_absolute_value=True),
                                  [('Ssb', q)], ['amax'])
                        P.add('dve', lambda e, N=N, q=q: e.tensor_tensor(out=Ssb[:, q, N - 256:N], in0=Ssb[:, q, N - 256:N], in1=cbf[:],
                                                                    op=ALU.add), [('Ssb', q), 'cbf'], [('Ssb', q)])
                        if i >= 1 and P3MODE >= 3:
                            P.add('dve', lambda e: e.tensor_scalar(out=W2[:, :], in0=pow2[:, :], scalar1=sm[:, 1:2], scalar2=None,
                                                                   op0=ALU.mult), ['amax', 'pow2'], ['W2'])
                            P.add('dve', lambda e: e.memset(sm[:, 2:3], 0.0), (), [('mid', 0)])
                            for k in range(NBIS):
                                a, b = 2 + (k % 2), 2 + ((k + 1) % 2)
                                P.add('dve', lambda e, N=N, a=a, q=q: e.tensor_scalar(
                                    out=cjunk[:, 0:N], in0=Ssb[:, q, 0:N], scalar1=sm[:, a:a + 1], scalar2=0.0,
                                    op0=ALU.is_ge, op1=ALU.add, accum_out=sm[:, 4:5]),
                                    [('Ssb', q), ('mid', k % 2)], ['cnt', 'cjunk'])
                                P.add('dve', lambda e: e.tensor_scalar(out=sm[:, 5:6], in0=sm[:, 4:5], scalar1=255.5, scalar2=0.5,
                                                                       op0=ALU.is_ge, op1=ALU.subtract), ['cnt'], ['dsg'])
                                P.add('dve', lambda e, a=a, b=b, k=k: e.scalar_tensor_tensor(
                                    out=sm[:, b:b + 1], in0=sm[:, 5:6], scalar=W2[:, k:k + 1], in1=sm[:, a:a + 1],
                                    op0=ALU.mult, op1=ALU.add), ['dsg', 'W2', ('mid', k % 2)], [('mid', (k + 1) % 2)])
                            fin = 2 + (NBIS % 2)
                            P.add('dve', lambda e, fin=fin: e.tensor_tensor(out=thr, in0=sm[:, fin:fin + 1],
                                                                            in1=W2[:, NBIS:NBIS + 1], op=ALU.subtract),
                                  [('mid', NBIS % 2), 'W2'], ['thr'])
                        else:
                            P.add('dve', lambda e: e.memset(thr, -1e29), (), ['thr'])
                        P.add('dve', lambda e, s=s, N=N, q=q: e.tensor_scalar(out=mbt[:, 0, 0:N], in0=Ssb[:, q, 0:N], scalar1=thr,
                                                                        scalar2=NEG, op0=ALU.is_lt, op1=ALU.mult),
                              [('Ssb', q), 'thr'], [('mbt', 0)])
                        P.add('sp', lambda e, s=s, i=i, N=N: e.dma_start(out=mbs[i, :, 0:N], in_=mbt[:, 0, 0:N]),
                              [('mbt', 0)], ['d_mb'], dma='smb0')
                    P.barrier()

                def attn_pass(kind, grp):
                    VW = 65 if kind == 'dsa' else 129
                    NV = 4 if kind == 'dsa' else 2
                    with contextlib.ExitStack() as st4:
                        def sb4(name, shape, dt):
                            return st4.enter_context(nc.sbuf_tensor(un(name), list(shape), dt))
                        kres = sb4("kres", [128, 2, S], BF)
                        vres = sb4("vres", [128, S // 128, NV, VW], BF)
                        qt = sb4("qt", [128, 2, 2, 2, 128], BF)
                        pT = sb4("pT", [128, 2, 8, 128], BF)
                        mix = sb4("mix", [128, 2, 256], BF)
                        fs = sb4("fs", [128, 2, 16], F32)
                        ftmp = sb4("ftmp", [128, 2, 128], F32)
                        mbt = sb4("mbt4", [128, 2, S], BF) if kind == 'dsa' else None
                        ubase = (0 if kind == 'dsa' else 2) + grp
                        qsrc = qaT if kind == 'dsa' else qbT
                        kview = kT_all_view(ubase)
                        kv5 = kres[:].rearrange("p c (l r k) -> p c l r k", r=2, k=128)
                        for r in range(2):
                            for c in range(2):
                                P.add('sp', lambda e, r=r, c=c: e.dma_start(
                                    out=kv5[:, c, :, r, :], in_=kview[r, :, c, :].rearrange("p (l k) -> p l k", k=128)),
                                    (), ['kres'], dma='kres')
                        P.add('pool', lambda e: e.memset(vres[:].rearrange("p a b c -> p (a b c)"), 1.0), (), ['vres'])
                        P.add('pool', lambda e: e.memset(qt[:].rearrange("p a b c d -> p (a b c d)"), 0.0), (), [('qt', 0, 0), ('qt', 0, 1), ('qt', 1, 0), ('qt', 1, 1)])
                        vsrc0 = 0 if kind == 'dsa' else 2
                        VLAST = 2 * (1 * (NT // 2) + (NT // 2 - 1)) + 1
                        c0 = grp * 256
                        for r in range(2):
                            for hu in range(2):
                                vv = v_all_view(vsrc0 + hu)[r]
                                nl = NT // 2
                                for l in range(nl):
                                    kt = 2 * (hu * nl + l) + r
                                    P.add('sp', lambda e, vv=vv, l=l, kt=kt: e.dma_start(
                                        out=vres[:, kt, :, 0:VW - 1],
                                        in_=vv[l * 128:(l + 1) * 128, c0:c0 + 256].rearrange("p (a b) -> p a b", a=NV)),
                                        ['vres'], [('vres', kt)], dma='vres')
                        for i in range(NT):
                            s = i % 2
                            t0 = i * 128
                            NK = 2 * i + 2
                            for e2_ in range(2):
                                P.add('sp', lambda e, s=s, t0=t0, e2_=e2_: e.dma_start(
                                    out=qt[64 * e2_:64 * e2_ + 64, s, :, e2_, :],
                                    in_=qsrc[64 * e2_:64 * e2_ + 64, 2 * grp:2 * grp + 2, t0:t0 + 128]),
                                    (), [('qt', s, e2_)], dma='qt%d' % s)
                            if kind == 'dsa':
                                P.add('sp', lambda e, s=s, i=i, NK=NK: e.dma_start(out=mbt[:, s, 0:NK * 128], in_=mbs[i, :, 0:NK * 128]),
                                      (), [('mbt', s)], dma='mbt%d' % s)
                            accb = 4 + 2 * s
                            NP = NK // 2

                            def rec_logits(kp, s=s, NP=NP):
                                ls = kp % 2
                                lb = 2 * ls
                                last = (kp == NP - 1)
                                for kl in range(2):
                                    kt = 2 * kp + kl
                                    need_mask = (kind == 'dsa') or last
                                    if need_mask:
                                        if kind == 'dsa':
                                            ml = mbt[:, s, kt * 128:(kt + 1) * 128]
                                            rr = [('mbt', s), 'ident4']
                                        else:
                                            ml = cbb[:, kl * 128:(kl + 1) * 128]
                                            rr = ['cbb', 'ident4']
                                        P.add('pe', lambda e, ml=ml, kl=kl, lb=lb: e.matmul(
                                            ps[:, lb + kl, :], lhsT=ml, rhs=ident4[:],
                                            start=True, stop=False, skip_group_check=True), rr, [('lg', ls)])
                                    for u in range(4):
                                        c, e2 = u // 2, u % 2
                                        P.add('pe', lambda e, c=c, e2=e2, kt=kt, kl=kl, u=u, lb=lb, need_mask=need_mask: e.matmul(
                                            ps[:, lb + kl, u * 128:(u + 1) * 128],
                                            lhsT=kres[:, c, kt * 128:(kt + 1) * 128],
                                            rhs=qt[:, s, c, e2, :], start=(not need_mask), stop=True, skip_group_check=True),
                                            ['kres', ('qt', s, 0), ('qt', s, 1)], [('lg', ls)])
                                P.add('act', lambda e, ls=ls, lb=lb: e.activation(
                                    out=pT[:, ls].rearrange("p a b -> p (a b)"),
                                    in_=ps[:, lb:lb + 2, :].rearrange("p a b -> p (a b)"), func=AF.Exp, scale=0.125),
                                    [('lg', ls)], [('pT', ls)])

                            def rec_av(kp, s=s, NK=NK, accb=accb):
                                ls = kp % 2
                                for kl in range(2):
                                    kt = 2 * kp + kl
                                    for u in range(4):
                                        vi = u if kind == 'dsa' else u // 2
                                        P.add('pe', lambda e, ls=ls, kl=kl, u=u, kt=kt, vi=vi: e.matmul(
                                            ps[:, accb + u // 2, (u % 2) * VW:(u % 2) * VW + VW],
                                            lhsT=pT[:, ls, kl * 4 + u, :], rhs=vres[:, kt, vi, :],
                                            start=(kt == 0 and u % 2 == 0), stop=(kt == NK - 1), skip_group_check=True),
                                            [('pT', ls), ('vres', VLAST), 'vres'], [('acc', s)])

                            rec_logits(0)
                            for kp in range(1, NP):
                                rec_logits(kp)
                                rec_av(kp - 1)
                            rec_av(NP - 1)
                            def accv(u, lo, hi):
                                return ps[:, accb + u // 2, (u % 2) * VW + lo:(u % 2) * VW + hi]
                            if kind == 'dsa':
                                for u in range(4):
                                    P.add('dve', lambda e, s=s, u=u: e.reciprocal(out=fs[:, s, u:u + 1], in_=accv(u, 64, 65)),
                                          [('acc', s)], [('fs', s, u)])
                                    P.add('dve', lambda e, s=s, u=u: e.tensor_scalar(out=mix[:, s, u * 64:(u + 1) * 64], in0=accv(u, 0, 64),
                                                                                    scalar1=fs[:, s, u:u + 1], scalar2=None, op0=ALU.mult),
                                          [('acc', s), ('fs', s, u)], [('mix', s)])
                            else:
                                for hl in range(2):
                                    u0, u1 = 2 * hl, 2 * hl + 1
                                    f = lambda k: fs[:, s, hl * 8 + k:hl * 8 + k + 1]
                                    P.add('dve', lambda e, f=f, u0=u0: e.reciprocal(out=f(0), in_=accv(u0, 128, 129)),
                                          [('acc', s)], [('fs', s, hl, 0)])
                                    P.add('dve', lambda e, f=f, u1=u1: e.reciprocal(out=f(1), in_=accv(u1, 128, 129)),
                                          [('acc', s)], [('fs', s, hl, 1)])
                                    P.add('dve', lambda e, f=f: e.tensor_tensor(out=f(2), in0=f(1), in1=neglam, op=ALU.mult),
                                          [('fs', s, hl, 1), 'neglam'], [('fs', s, hl, 2)])
                                    P.add('dve', lambda e, f=f, u1=u1, s=s: e.tensor_scalar(out=ftmp[:, s, :], in0=accv(u1, 0, 128), scalar1=f(2),
                                                                                           scalar2=None, op0=ALU.mult),
                                          [('acc', s), ('fs', s, hl, 2)], [('ftmp', s)])
                                    P.add('dve', lambda e, f=f, u0=u0, s=s: e.scalar_tensor_tensor(out=ftmp[:, s, :], in0=accv(u0, 0, 128), scalar=f(0),
                                                                                                  in1=ftmp[:, s, :], op0=ALU.mult, op1=ALU.add),
                                          [('acc', s), ('fs', s, hl, 0), ('ftmp', s)], [('ftmp2', s)])
                                    P.add('dve', lambda e, f=f, s=s: e.scalar_tensor_tensor(out=junk[:, 0:128], in0=ftmp[:, s, :], scalar=1.0,
                                                                                           in1=ftmp[:, s, :], op0=ALU.mult, op1=ALU.mult,
                                                                                           accum_out=f(3)),
                                          [('ftmp2', s)], ['junk', ('fs', s, hl, 3)])
                                    P.add('dve', lambda e, f=f: e.tensor_scalar(out=f(4), in0=f(3), scalar1=1.0 / 128, scalar2=EPS,
                                                                                op0=ALU.mult, op1=ALU.add),
                                          [('fs', s, hl, 3)], [('fs', s, hl, 4)])
                                    P.add('pool', lambda e, f=f: e.tensor_tensor(out=f(5), in0=f(4), in1=mhalf[:, 0:1], op=ALU.pow),
                                          [('fs', s, hl, 4), 'mhalf'], [('fs', s, hl, 5)])
                                    P.add('dve', lambda e, f=f, s=s, hl=hl: e.scalar_tensor_tensor(
                                        out=mix[:, s, hl * 128:(hl + 1) * 128], in0=ftmp[:, s, :], scalar=f(5), in1=gsub[:],
                                        op0=ALU.mult, op1=ALU.mult), [('ftmp2', s), ('fs', s, hl, 5), 'gsub'], [('mix', s)])
                            mc0 = (0 if kind == 'dsa' else 512) + grp * 256
                            P.add('sp', lambda e, s=s, t0=t0, mc0=mc0: e.dma_start(out=mixed[t0:t0 + 128, mc0:mc0 + 256], in_=mix[:, s, :]),
                                  [('mix', s)], ['d_mixed'], dma='smix%d' % s)
                    P.barrier()

                for kind in ('dsa', 'diff'):
                    for grp in range(2):
                        attn_pass(kind, grp)

                with contextlib.ExitStack() as st6:
                    def sb6(name, shape, dt):
                        return st6.enter_context(nc.sbuf_tensor(un(name), list(shape), dt))
                    wo = sb6("wo", [128, 8, D], BF)
                    wst = sb6("wst6", [128, 2, 842], F32)
                    xt = sb6("xt6", [128, 2, D], F32)
                    mt = sb6("mt6", [128, 2, D], BF)
                    mT = sb6("mT6", [128, 2, 8, 128], BF)
                    for kc in range(8):
                        load_weight(wo[:, kc, :], w_out[L, kc * 128:(kc + 1) * 128, :], D, wst, 'wo', chunk=512)
                    for it in range(NT):
                        s = it % 2
                        t0 = it * 128
                        P.add('sp', lambda e, s=s, t0=t0: e.dma_start(out=xt[:, s, :], in_=xsrc[t0:t0 + 128, :]),
                              (), [('xt', s)], dma='xt%d' % s)
                        P.add('sp', lambda e, s=s, t0=t0: e.dma_start(out=mt[:, s, :], in_=mixed[t0:t0 + 128, :]),
                              (), [('mt', s)], dma='mt%d' % s)
                        for kc in range(8):
                            P.add('pe', lambda e, s=s, kc=kc: e.transpose(out=psb(0)[:, kc * 128:(kc + 1) * 128],
                                                                          in_=mt[:, s, kc * 128:(kc + 1) * 128], identity=ident[:]),
                                  [('mt', s), 'ident'], ['pb0'])
                        P.add('act', lambda e, s=s: e.copy(out=mT[:, s].rearrange("p a b -> p (a b)"), in_=psb(0)[:, :]),
                              ['pb0'], [('mT', s)])
                        pb = 2 + 2 * s
                        for cg in range(2):
                            for kc in range(8):
                                P.add('pe', lambda e, s=s, kc=kc, cg=cg, pb=pb: e.matmul(
                                    ps[:, pb + cg, :], lhsT=mT[:, s, kc, :], rhs=wo[:, kc, cg * 512:(cg + 1) * 512],
                                    start=(kc == 0), stop=(kc == 7)), [('mT', s), 'wo'], [('po', s)])
                        P.add('dve', lambda e, s=s, pb=pb: e.tensor_tensor(out=xt[:, s, :], in0=ps[:, pb:pb + 2, :].rearrange("p a b -> p (a b)"),
                                                                           in1=xt[:, s, :], op=ALU.add),
                              [('po', s), ('xt', s)], [('x1', s)])
                        P.add('dve', lambda e, s=s, it=it: e.scalar_tensor_tensor(out=junk[:, :], in0=xt[:, s, :], scalar=1.0, in1=xt[:, s, :],
                                                                                  op0=ALU.mult, op1=ALU.mult, accum_out=ssA[:, it:it + 1]),
                              [('x1', s), ('xt', s)], ['junk', 'ss'])
                        P.add('sp', lambda e, s=s, t0=t0: e.dma_start(out=xres[t0:t0 + 128, :], in_=xt[:, s, :]),
                              [('x1', s)], ['d_xres', ('xt', s)], dma='sx%d' % s)
                    rstd_from_ss(ssA[:, :], rstd[:, :], NT, 1.0 / D)
                    P.barrier()

                with contextlib.ExitStack() as st7:
                    def sb7(name, shape, dt):
                        return st7.enter_context(nc.sbuf_tensor(un(name), list(shape), dt))
                    wg = sb7("wg", [128, 8, DFF], BF)
                    wu = sb7("wu", [128, 8, DFF], BF)
                    wd = sb7("wd", [128, 22, D], BF)
                    wst = sb7("wst7", [128, 2, 512], F32)
                    gbc = sb7("gbc7", [128, D], F32)
                    gfc = sb7("gfc7", [128, D], F32)
                    xt = sb7("xt7", [128, 2, D], F32)
                    hb = sb7("hb7", [128, D], BF)
                    hT = sb7("hT7", [128, 8, 128], BF)
                    sg = sb7("sg7", [128, 2, 512], F32)
                    gb = sb7("gb7", [128, DFF], BF)
                    gT = sb7("gT7", [128, 22, 128], BF)
                    rs2 = sb7("rs27", [128, 2, 4], F32)
                    P.add('sp', lambda e, L=L: e.dma_start(out=gbc[:], in_=g_ffn[L:L + 1, :].partition_broadcast(128)),
                          (), ['gbc'], dma='c2')
                    P.add('sp', lambda e: e.dma_start(out=gfc[:], in_=g_final[0:1, :].partition_broadcast(128)),
                          (), ['gfc'], dma='c3')
                    for kc in range(8):
                        load_weight(wg[:, kc, :], w_gate[L, kc * 128:(kc + 1) * 128, :], DFF, wst, 'wg', chunk=512)
                        load_weight(wu[:, kc, :], w_up[L, kc * 128:(kc + 1) * 128, :], DFF, wst, 'wu', chunk=512)
                    for kc in range(22):
                        load_weight(wd[:, kc, :], w_down[L, kc * 128:(kc + 1) * 128, :], D, wst, 'wd', chunk=512)
                    for it in range(NT):
                        s = it % 2
                        t0 = it * 128
                        P.add('sp', lambda e, s=s, t0=t0: e.dma_start(out=xt[:, s, :], in_=xres[t0:t0 + 128, :]),
                              ['d_xres'], [('xt', s)], dma='xt%d' % s)
                        P.add('dve', lambda e, s=s, it=it: e.scalar_tensor_tensor(out=hb[:, :], in0=xt[:, s, :], scalar=rstd[:, it:it + 1],
                                                                                  in1=gbc[:], op0=ALU.mult, op1=ALU.mult),
                              [('xt', s), 'rstd', 'gbc'], ['hb'])
                        for kc in range(8):
                            P.add('pe', lambda e, kc=kc: e.transpose(out=psb(0)[:, kc * 128:(kc + 1) * 128],
                                                                     in_=hb[:, kc * 128:(kc + 1) * 128], identity=ident[:]),
                                  ['hb', 'ident'], [('pb', 0)])
                        P.add('act', lambda e: e.copy(out=hT[:].rearrange("p a b -> p (a b)"), in_=psb(0)[:, :]),
                              [('pb', 0)], ['hT'])
                        for cg in range(6):
                            c0 = cg * 512
                            n = min(512, DFF - c0)
                            q = cg % 2
                            bg, bu = 2 + 2 * q, 3 + 2 * q
                            for kc in range(8):
                                P.add('pe', lambda e, kc=kc, c0=c0, n=n, bg=bg: e.matmul(ps[:, bg, 0:n], lhsT=hT[:, kc, :], rhs=wg[:, kc, c0:c0 + n],
                                                                                         start=(kc == 0), stop=(kc == 7)), ['hT', 'wg'], [('pb', bg)])
                            for kc in range(8):
                                P.add('pe', lambda e, kc=kc, c0=c0, n=n, bu=bu: e.matmul(ps[:, bu, 0:n], lhsT=hT[:, kc, :], rhs=wu[:, kc, c0:c0 + n],
                                                                                         start=(kc == 0), stop=(kc == 7)), ['hT', 'wu'], [('pb', bu)])
                            P.add('act', lambda e, q=q, n=n, bg=bg: e.activation(out=sg[:, q, 0:n], in_=ps[:, bg, 0:n], func=AF.Silu),
                                  [('pb', bg)], [('sg', q)])
                            P.add('dve', lambda e, q=q, n=n, bu=bu, c0=c0: e.tensor_tensor(out=gb[:, c0:c0 + n], in0=ps[:, bu, 0:n], in1=sg[:, q, 0:n],
                                                                                          op=ALU.mult), [('pb', bu), ('sg', q)], ['gb'])
                        for rnd in range(3):
                            lo, hi = rnd * 8, min(rnd * 8 + 8, 22)
                            tb = rnd % 2
                            for k in range(lo, hi):
                                P.add('pe', lambda e, k=k, lo=lo, tb=tb: e.transpose(out=psb(tb)[:, (k - lo) * 128:(k - lo + 1) * 128],
                                                                                     in_=gb[:, k * 128:(k + 1) * 128], identity=ident[:]),
                                      ['gb', 'ident'], [('pb', tb)])
                            P.add('act', lambda e, lo=lo, hi=hi, tb=tb: e.copy(out=gT[:, lo:hi, :].rearrange("p a b -> p (a b)"),
                                                                               in_=psb(tb)[:, 0:(hi - lo) * 128]), [('pb', tb)], ['gT'])
                        for cg in range(2):
                            for kc in range(22):
                                P.add('pe', lambda e, kc=kc, cg=cg: e.matmul(ps[:, 6 + cg, :], lhsT=gT[:, kc, :], rhs=wd[:, kc, cg * 512:(cg + 1) * 512],
                                                                             start=(kc == 0), stop=(kc == 21)), ['gT', 'wd'], ['pdn'])
                        P.add('dve', lambda e, s=s: e.tensor_tensor(out=xt[:, s, :], in0=ps[:, 6:8, :].rearrange("p a b -> p (a b)"),
                                                                    in1=xt[:, s, :], op=ALU.add), ['pdn', ('xt', s)], [('x2', s)])
                        if L < DEPTH - 1:
                            P.add('dve', lambda e, s=s, it=it: e.scalar_tensor_tensor(out=junk[:, :], in0=xt[:, s, :], scalar=1.0, in1=xt[:, s, :],
                                                                                      op0=ALU.mult, op1=ALU.mult, accum_out=ssA[:, it:it + 1]),
                                  [('x2', s), ('xt', s)], ['junk', 'ss2'])
                            P.add('sp', lambda e, s=s, t0=t0: e.dma_start(out=xres[t0:t0 + 128, :], in_=xt[:, s, :]),
                                  [('x2', s)], ['d_xres2', ('xt', s)], dma='sx%d' % s)
                        else:
                            P.add('dve', lambda e, s=s: e.scalar_tensor_tensor(out=junk[:, :], in0=xt[:, s, :], scalar=1.0, in1=xt[:, s, :],
                                                                               op0=ALU.mult, op1=ALU.mult, accum_out=rs2[:, s, 0:1]),
                                  [('x2', s), ('xt', s)], ['junk', ('rs2', s, 0)])
                            P.add('dve', lambda e, s=s: e.tensor_scalar(out=rs2[:, s, 1:2], in0=rs2[:, s, 0:1], scalar1=1.0 / D, scalar2=EPS,
                                                                        op0=ALU.mult, op1=ALU.add), [('rs2', s, 0)], [('rs2', s, 1)])
                            P.add('pool', lambda e, s=s: e.tensor_tensor(out=rs2[:, s, 2:3], in0=rs2[:, s, 1:2], in1=mhalf[:, 0:1], op=ALU.pow),
                                  [('rs2', s, 1), 'mhalf'], [('rs2', s, 2)])
                            P.add('dve', lambda e, s=s: e.scalar_tensor_tensor(out=xt[:, s, :], in0=xt[:, s, :], scalar=rs2[:, s, 2:3], in1=gfc[:],
                                                                               op0=ALU.mult, op1=ALU.mult),
                                  [('x2', s), ('rs2', s, 2), 'gfc'], [('yo', s)])
                            P.add('sp', lambda e, s=s, t0=t0: e.dma_start(out=y_out[t0:t0 + 128, :], in_=xt[:, s, :]),
                                  [('yo', s)], ['d_y', ('xt', s)], dma='sx%d' % s)
                    if L < DEPTH - 1:
                        rstd_from_ss(ssA[:, :], rstd[:, :], NT, 1.0 / D)
                    P.barrier()

        except _Stop:
            pass
        P.emit(nc, stack)
    return nc


def _consts(S, r):
    TPC = S // 2
    NT = TPC // 128
    pos = np.concatenate([np.arange(128) + (2 * i + r) * 128 for i in range(NT)]).astype(np.float32)
    def tab(dim):
        inv = (10000.0 ** (-np.arange(0, dim, 2, dtype=np.float32) / dim)).astype(np.float32)
        ang = pos[:, None] * inv[None, :]
        return np.cos(ang).astype(np.float32), np.sin(ang).astype(np.float32)
    c64, s64 = tab(64)
    c32, s32 = tab(32)
    rope = np.concatenate([c64, s64, c32, s32], axis=1).astype(np.float32)
    t = np.arange(128)[:, None] // 64
    s_ = np.arange(128)[None, :] // 64
    diag = (s_ <= t)
    full = np.ones((128, 128), bool)
    none = np.zeros((128, 128), bool)
    allow = np.concatenate([diag, none], axis=1) if r == 0 else np.concatenate([full, diag], axis=1)
    cbf = np.where(allow, 0.0, -1e30).astype(np.float32)
    cbb = np.where(allow, 0.0, NEG).astype(np.float32).astype(ml_dtypes.bfloat16)
    ident = np.eye(128, dtype=np.float32).astype(ml_dtypes.bfloat16)
    pow2 = np.tile((2.0 ** -np.arange(32, dtype=np.float32))[None, :], (128, 1)).astype(np.float32)
    return dict(rope=rope, cbf=cbf, cbb=cbb, ident=ident, pow2=pow2)


_NC_CACHE = {}


def _prep(x, w_in, w_out, g_mix, lam_q1, lam_k1, lam_q2, lam_k2, g_subln, g_ffn, w_gate, w_up, w_down, g_final):
    x = np.asarray(x, dtype=np.float32)
    B, S, _ = x.shape
    assert B == 4
    TPC = S // 2
    f = lambda a: np.ascontiguousarray(np.asarray(a, dtype=np.float32))
    lamv = np.ascontiguousarray(np.stack([f(lam_q1), f(lam_k1), f(lam_q2), f(lam_k2)], axis=1))
    shared = dict(w_in=f(w_in), w_out=f(w_out), w_gate=f(w_gate), w_up=f(w_up), w_down=f(w_down), g_mix=f(g_mix),
                  g_ffn=f(g_ffn), g_final=f(g_final).reshape(1, D), g_subln=f(g_subln), lamv=lamv)
    in_maps = []
    for c in range(8):
        b, r = c // 2, c % 2
        xb = x[b].reshape(S // 128, 128, D)
        xl = np.ascontiguousarray(xb[r::2].reshape(TPC, D))
        m = dict(shared)
        m["x"] = xl
        m.update(_consts(S, r))
        in_maps.append(m)
    return in_maps, B, S


def _gather(ys, B, S):
    NT = S // 256
    out = np.empty((B, S, D), dtype=np.float32)
    for c in range(8):
        b, r = c // 2, c % 2
        yl = np.asarray(ys[c], dtype=np.float32).reshape(NT, 128, D)
        out[b].reshape(S // 128, 128, D)[r::2] = yl
    return out


def kernel(x, w_in, w_out, g_mix, lam_q1, lam_k1, lam_q2, lam_k2, g_subln, g_ffn, w_gate, w_up, w_down, g_final):
    in_maps, B, S = _prep(x, w_in, w_out, g_mix, lam_q1, lam_k1, lam_q2, lam_k2, g_subln, g_ffn, w_gate, w_up, w_down, g_final)
    if S not in _NC_CACHE:
        _NC_CACHE[S] = build(S)
    nc = _NC_CACHE[S]
    res = run_bass_kernel_spmd(nc, in_maps, core_ids=list(range(8)))
    return _gather([res.results[c]["y"] for c in range(8)], B, S)
```
